# Optimizing a Trainium2 kernel written in Bass

```python
import math
import jax
import jax.numpy as jnp
from jax import lax
import numpy as np

D_MODEL = 1024
BATCH = 8
SEQ = 4096
DEPTH = 2

GRID_W = 64
CTX_LEN = 256
EPS = 1e-6
CHUNK = 64

GLA_HEADS = 4
GLA_DK = 64
GLA_DV = 128
GLA_RANK = 16
GLA_GATE_NORM = 16.0
GLA_QK = GLA_HEADS * GLA_DK
GLA_V = GLA_HEADS * GLA_DV
S5_WIDTH = D_MODEL - GLA_V
S5_GROUP = 16
S5_GROUPS = S5_WIDTH // S5_GROUP
S5_STATE = 64
S5_DT_MIN = 1e-3
S5_DT_MAX = 1e-1
AB_SPLITS = (GLA_QK, 2 * GLA_QK, 2 * GLA_QK + GLA_V, 2 * GLA_QK + 2 * GLA_V,
             2 * GLA_QK + 2 * GLA_V + GLA_RANK, 2 * GLA_QK + 2 * GLA_V + 2 * GLA_RANK)
AB_IN = AB_SPLITS[-1] + S5_WIDTH
HG_EXPAND = 128
HG_HEADS = D_MODEL // HG_EXPAND
HG_DV = D_MODEL // HG_HEADS
N_EXPERTS = 32
N_GROUPS = 8
EXPERTS_PER_GROUP = N_EXPERTS // N_GROUPS
TOP_K = 2
D_EXPERT = 256

N_EVEN = (DEPTH + 1) // 2
N_ODD = DEPTH // 2

kernel_name = 'hybrid_gla_s5_hgrn2_groupmoe_diffusion'

F32 = jnp.float32


def rmsnorm(x, g):
    xf = x.astype(F32)
    y = xf * lax.rsqrt(jnp.mean(xf * xf, axis=-1, keepdims=True) + EPS)
    return y.astype(x.dtype) * g


def to_heads(t, n_heads):
    b, n, w = t.shape
    return t.reshape(b, n, n_heads, w // n_heads).transpose(0, 2, 1, 3)


def from_heads(t):
    b, h, n, d = t.shape
    return t.transpose(0, 2, 1, 3).reshape(b, n, h * d)


def grid_sincos(n_tokens, dim):
    rows = n_tokens // GRID_W
    r, col = jnp.meshgrid(jnp.arange(rows, dtype=F32), jnp.arange(GRID_W, dtype=F32), indexing='ij')
    quarter = dim // 4
    omega = 1.0 / (10000.0 ** (jnp.arange(quarter, dtype=F32) / quarter))
    def emb(pos):
        ang = pos.reshape(-1, 1) * omega
        return jnp.concatenate([jnp.sin(ang), jnp.cos(ang)], axis=-1)
    return jnp.concatenate([emb(r), emb(col)], axis=-1)


def chunked_gated_recurrence(q, k, v, log_a, h0):
    out_dtype = v.dtype
    q, k, v, log_a = (t.astype(F32) for t in (q, k, v, log_a))
    bsz, nh, n, dk = q.shape
    dv = v.shape[-1]
    nc = n // CHUNK
    rs = lambda t: t.reshape(bsz, nh, nc, CHUNK, t.shape[-1])
    q, k, v, log_a = rs(q), rs(k), rs(v), rs(log_a)
    b = jnp.cumsum(log_a, axis=3)
    b_mid = b[:, :, :, CHUNK // 2 - 1:CHUNK // 2, :]
    b_end = b[:, :, :, -1:, :]
    attn = jnp.einsum('bhclk,bhcsk->bhcls', q * jnp.exp(b - b_mid), k * jnp.exp(b_mid - b))
    attn = jnp.where(jnp.tril(jnp.ones((CHUNK, CHUNK), dtype=bool)), attn, 0.0)
    o_intra = jnp.einsum('bhcls,bhcsv->bhclv', attn, v)
    q_in = jnp.moveaxis(q * jnp.exp(b), 2, 0)
    d_state = jnp.moveaxis(jnp.einsum('bhcsk,bhcsv->bhckv', k * jnp.exp(b_end - b), v), 2, 0)
    decay = jnp.moveaxis(jnp.exp(b_end[:, :, :, 0, :]), 2, 0)
    s0 = jnp.zeros((bsz, nh, dk, dv), F32) if h0 is None else h0.astype(F32)

    def step(s, xs):
        qc, dsc, dc = xs
        oc = jnp.einsum('bhlk,bhkv->bhlv', qc, s)
        return dc[..., None] * s + dsc, oc

    s_fin, o_inter = lax.scan(step, s0, (q_in, d_state, decay))
    o = o_intra + jnp.moveaxis(o_inter, 0, 2)
    return o.reshape(bsz, nh, n, dv).astype(out_dtype), s_fin


def gated_final_state(k, v, log_a):
    b = jnp.cumsum(log_a.astype(F32), axis=2)
    w = jnp.exp(b[:, :, -1:, :] - b)
    return jnp.einsum('bhnk,bhnv->bhkv', k.astype(F32) * w, v.astype(F32))


def bidir_gated_recurrence(lat, ctx, need_ctx):
    q, k_f, k_b, v, la_f, la_b = lat
    cq, ck_f, ck_b, cv, cla_f, cla_b = ctx
    fl = lambda t: jnp.flip(t, axis=2)
    if need_ctx:
        co_f, s_f = chunked_gated_recurrence(cq, ck_f, cv, cla_f, None)
        co_b, s_b = chunked_gated_recurrence(fl(cq), fl(ck_b), fl(cv), fl(cla_b), None)
        co = co_f + fl(co_b)
    else:
        s_f = gated_final_state(ck_f, cv, cla_f)
        s_b = gated_final_state(fl(ck_b), fl(cv), fl(cla_b))
        co = None
    o_f, _ = chunked_gated_recurrence(q, k_f, v, la_f, s_f)
    o_b, _ = chunked_gated_recurrence(fl(q), fl(k_b), fl(v), fl(la_b), s_b)
    return o_f + fl(o_b), co


def s5_discretize(lam_re, lam_im, log_dt, b_re, b_im):
    dt = jnp.exp(log_dt)[:, None]
    mag = jnp.exp(lam_re * dt)
    a_re, a_im = mag * jnp.cos(lam_im * dt), mag * jnp.sin(lam_im * dt)
    den = lam_re * lam_re + lam_im * lam_im
    f_re = ((a_re - 1.0) * lam_re + a_im * lam_im) / den
    f_im = (a_im * lam_re - (a_re - 1.0) * lam_im) / den
    bb_re = f_re[..., None] * b_re - f_im[..., None] * b_im
    bb_im = f_re[..., None] * b_im + f_im[..., None] * b_re
    return a_re, a_im, bb_re, bb_im


def s5_scan(u, a_re, a_im, bb_re, bb_im, h0, reverse):
    x_re = jnp.einsum('nbgh,gph->nbgp', u, bb_re)
    x_im = jnp.einsum('nbgh,gph->nbgp', u, bb_im)
    if h0 is not None:
        h_re, h_im = h0
        edge = -1 if reverse else 0
        x_re = x_re.at[edge].add(a_re * h_re - a_im * h_im)
        x_im = x_im.at[edge].add(a_re * h_im + a_im * h_re)
    n = u.shape[0]
    ar = jnp.broadcast_to(a_re[None, None], (n, 1) + a_re.shape)
    ai = jnp.broadcast_to(a_im[None, None], (n, 1) + a_im.shape)

    def combine(e1, e2):
        a1r, a1i, b1r, b1i = e1
        a2r, a2i, b2r, b2i = e2
        return (a2r * a1r - a2i * a1i, a2r * a1i + a2i * a1r,
                a2r * b1r - a2i * b1i + b2r, a2r * b1i + a2i * b1r + b2i)

    _, _, s_re, s_im = lax.associative_scan(combine, (ar, ai, x_re, x_im), reverse=reverse, axis=0)
    return s_re, s_im


def s5_bidir(u, uc, lam_re, lam_im, log_dt, b_re, b_im, c_re, c_im, d, need_ctx):
    def to_scan(t):
        bsz, n, _ = t.shape
        return t.astype(F32).reshape(bsz, n, S5_GROUPS, S5_GROUP).transpose(1, 0, 2, 3)

    def readout(s_re, s_im, cr, ci):
        y = jnp.einsum('nbgp,ghp->bngh', s_re, cr) - jnp.einsum('nbgp,ghp->bngh', s_im, ci)
        return y.reshape(y.shape[0], y.shape[1], S5_WIDTH)

    us, ucs = to_scan(u), to_scan(uc)
    y = d.astype(F32) * u.astype(F32)
    yc = d.astype(F32) * uc.astype(F32) if need_ctx else None
    for di, rev in enumerate((False, True)):
        a_re, a_im, bb_re, bb_im = s5_discretize(lam_re[di].astype(F32), lam_im[di].astype(F32),
                                                 log_dt[di].astype(F32), b_re[di].astype(F32),
                                                 b_im[di].astype(F32))
        sc_re, sc_im = s5_scan(ucs, a_re, a_im, bb_re, bb_im, None, rev)
        edge = 0 if rev else -1
        s_re, s_im = s5_scan(us, a_re, a_im, bb_re, bb_im, (sc_re[edge], sc_im[edge]), rev)
        cr, ci = c_re[di].astype(F32), c_im[di].astype(F32)
        y = y + readout(s_re, s_im, cr, ci)
        if need_ctx:
            yc = yc + readout(sc_re, sc_im, cr, ci)
    return y.astype(u.dtype), (yc.astype(uc.dtype) if need_ctx else None)


def mixer_gla_s5(h, hc, w_in, w_out, gla_a2, gla_ab, gla_norm, lam_re, lam_im, log_dt,
                 b_re, b_im, c_re, c_im, s5_d, glu_w, glu_b, need_ctx):
    def project(t):
        q, k, v, g, a_f, a_b, u = jnp.split(t @ w_in, AB_SPLITS, axis=-1)
        la_f = jax.nn.log_sigmoid((a_f @ gla_a2[0] + gla_ab[0]).astype(F32)) / GLA_GATE_NORM
        la_b = jax.nn.log_sigmoid((a_b @ gla_a2[1] + gla_ab[1]).astype(F32)) / GLA_GATE_NORM
        kh = to_heads(k, GLA_HEADS)
        heads = (to_heads(q * GLA_DK ** -0.5, GLA_HEADS), kh, kh, to_heads(v, GLA_HEADS),
                 to_heads(la_f, GLA_HEADS), to_heads(la_b, GLA_HEADS))
        return heads, g, u

    lat, g, u = project(h)
    ctx_heads, gc, uc = project(hc)
    o, oc = bidir_gated_recurrence(lat, ctx_heads, need_ctx)
    s, sc = s5_bidir(u, uc, lam_re, lam_im, log_dt, b_re, b_im, c_re, c_im, s5_d, need_ctx)
    gla_out = lambda o_, g_: from_heads(rmsnorm(o_, gla_norm)) * jax.nn.silu(g_)

    def glu(s_):
        a = jax.nn.gelu(s_)
        return a * jax.nn.sigmoid(a @ glu_w + glu_b)

    y = jnp.concatenate([gla_out(o, g), glu(s)], axis=-1) @ w_out
    yc = jnp.concatenate([gla_out(oc, gc), glu(sc)], axis=-1) @ w_out if need_ctx else None
    return y, yc


def mixer_hgrn2(h, hc, w_in, w_out, lb, hg_norm, need_ctx):
    def project(t):
        q, f_f, f_b, i, g = jnp.split(t @ w_in, 5, axis=-1)

        def gate(f_pre):
            f = lb + (1.0 - lb) * jax.nn.sigmoid(f_pre.astype(F32))
            return to_heads(1.0 - f, HG_HEADS), to_heads(jnp.log(f), HG_HEADS)

        k_f, la_f = gate(f_f)
        k_b, la_b = gate(f_b)
        heads = (to_heads(jax.nn.silu(q), HG_HEADS), k_f, k_b, to_heads(i, HG_HEADS), la_f, la_b)
        return heads, g

    lat, g = project(h)
    ctx_heads, gc = project(hc)
    o, oc = bidir_gated_recurrence(lat, ctx_heads, need_ctx)
    out = lambda o_, g_: (from_heads(rmsnorm(o_, hg_norm)) * jax.nn.silu(g_)) @ w_out
    return out(o, g), (out(oc, gc) if need_ctx else None)


def grouped_moe(t, router_w, router_bias, w_gate, w_up, w_down):
    aff = jax.nn.sigmoid((t @ router_w).astype(F32))
    sel = aff + router_bias.astype(F32)
    sel_g = sel.reshape(-1, N_GROUPS, EXPERTS_PER_GROUP)
    group_score = jnp.sum(lax.top_k(sel_g, TOP_K)[0], axis=-1)
    grp = jnp.argmax(group_score, axis=-1)
    in_grp = jnp.take_along_axis(sel_g, grp[:, None, None], axis=1)[:, 0]
    _, local = lax.top_k(in_grp, TOP_K)
    idx = grp[:, None] * EXPERTS_PER_GROUP + local
    w = jnp.take_along_axis(aff, idx, axis=1)
    w = w / jnp.sum(w, axis=-1, keepdims=True)
    gates = jnp.sum(jax.nn.one_hot(idx, N_EXPERTS, dtype=F32) * w[..., None], axis=1).astype(t.dtype)

    def expert(acc, p):
        wg, wu, wd, ge = p
        y = (jax.nn.silu(t @ wg) * (t @ wu)) @ wd
        return acc + ge[:, None] * y, None

    out, _ = lax.scan(expert, jnp.zeros_like(t), (w_gate, w_up, w_down, gates.T))
    return out


def setup_inputs(seed: int = 0) -> dict:
    key = jax.random.key(seed)
    ks = iter(jax.random.split(key, 40))
    D = D_MODEL
    nrm = lambda shape, scale: scale * jax.random.normal(next(ks), shape, F32)
    gain = lambda shape: 1.0 + nrm(shape, 0.02)
    s5_shape = (N_EVEN, 2, S5_GROUPS, S5_STATE)
    n_idx = jnp.arange(S5_STATE, dtype=F32)
    return {
        'x': nrm((BATCH, SEQ, D), 1.0),
        'c': nrm((BATCH, D), 1.0),
        'ctx': nrm((BATCH, CTX_LEN, D), 1.0),
        'c_ctx': nrm((D,), 1.0),
        'ada_w': nrm((DEPTH, D, 6 * D), 0.5 * D ** -0.5),
        'ada_b': nrm((DEPTH, 6 * D), 0.02),
        'norm_mix': gain((DEPTH, D)),
        'norm_ffn': gain((DEPTH, D)),
        'ab_w_in': nrm((N_EVEN, D, AB_IN), D ** -0.5),
        'ab_w_out': nrm((N_EVEN, GLA_V + S5_WIDTH, D), (GLA_V + S5_WIDTH) ** -0.5),
        'gla_a2': nrm((N_EVEN, 2, GLA_RANK, GLA_QK), GLA_RANK ** -0.5),
        'gla_ab': nrm((N_EVEN, 2, GLA_QK), 0.1),
        'gla_norm': gain((N_EVEN, GLA_DV)),
        's5_lam_re': -0.5 + nrm(s5_shape, 0.01),
        's5_lam_im': math.pi * n_idx + nrm(s5_shape, 0.01),
        's5_log_dt': jax.random.uniform(next(ks), (N_EVEN, 2, S5_GROUPS), F32,
                                        math.log(S5_DT_MIN), math.log(S5_DT_MAX)),
        's5_b_re': nrm((N_EVEN, 2, S5_GROUPS, S5_STATE, S5_GROUP), (2 * S5_GROUP) ** -0.5),
        's5_b_im': nrm((N_EVEN, 2, S5_GROUPS, S5_STATE, S5_GROUP), (2 * S5_GROUP) ** -0.5),
        's5_c_re': nrm((N_EVEN, 2, S5_GROUPS, S5_GROUP, S5_STATE), 0.5),
        's5_c_im': nrm((N_EVEN, 2, S5_GROUPS, S5_GROUP, S5_STATE), 0.5),
        's5_d': nrm((N_EVEN, S5_WIDTH), 0.5),
        's5_glu_w': nrm((N_EVEN, S5_WIDTH, S5_WIDTH), S5_WIDTH ** -0.5),
        's5_glu_b': nrm((N_EVEN, S5_WIDTH), 0.01),
        'hg_w_in': nrm((N_ODD, D, 5 * D), D ** -0.5),
        'hg_w_out': nrm((N_ODD, D, D), D ** -0.5),
        'hg_lb_logits': nrm((DEPTH, D), 0.1),
        'hg_norm': gain((N_ODD, HG_DV)),
        'router_w': nrm((D, N_EXPERTS), D ** -0.5),
        'router_bias': nrm((N_EXPERTS,), 0.01),
        'moe_w_gate': nrm((DEPTH, N_EXPERTS, D, D_EXPERT), D ** -0.5),
        'moe_w_up': nrm((DEPTH, N_EXPERTS, D, D_EXPERT), D ** -0.5),
        'moe_w_down': nrm((DEPTH, N_EXPERTS, D_EXPERT, D), D_EXPERT ** -0.5),
        'final_norm': gain((D,)),
    }


def reference(x, c, ctx, c_ctx, ada_w, ada_b, norm_mix, norm_ffn, ab_w_in, ab_w_out,
              gla_a2, gla_ab, gla_norm, s5_lam_re, s5_lam_im, s5_log_dt, s5_b_re, s5_b_im,
              s5_c_re, s5_c_im, s5_d, s5_glu_w, s5_glu_b, hg_w_in, hg_w_out, hg_lb_logits,
              hg_norm, router_w, router_bias, moe_w_gate, moe_w_up, moe_w_down, final_norm):
    n_lat_seq, d = x.shape[1], x.shape[2]
    x = x + grid_sincos(n_lat_seq, d).astype(x.dtype)[None]
    cx = ctx
    lb_all = jax.nn.softmax(hg_lb_logits.astype(F32), axis=0)
    lb_all = jnp.cumsum(lb_all, axis=0) - lb_all[0]
    silu_c = jax.nn.silu(c)
    silu_cc = jax.nn.silu(c_ctx)
    for l in range(DEPTH):
        last = l == DEPTH - 1
        j = l // 2
        mod = silu_c @ ada_w[l] + ada_b[l]
        mod_c = silu_cc @ ada_w[l] + ada_b[l]
        sh1, sc1, g1, sh2, sc2, g2 = jnp.split(mod[:, None, :], 6, axis=-1)
        csh1, csc1, cg1, csh2, csc2, cg2 = jnp.split(mod_c, 6)
        h = rmsnorm(x, norm_mix[l]) * (1.0 + sc1) + sh1
        hc = rmsnorm(cx, norm_mix[l]) * (1.0 + csc1) + csh1
        if l % 2 == 0:
            y, yc = mixer_gla_s5(h, hc, ab_w_in[j], ab_w_out[j], gla_a2[j], gla_ab[j], gla_norm[j],
                                 s5_lam_re[j], s5_lam_im[j], s5_log_dt[j], s5_b_re[j], s5_b_im[j],
                                 s5_c_re[j], s5_c_im[j], s5_d[j], s5_glu_w[j], s5_glu_b[j],
                                 need_ctx=not last)
        else:
            y, yc = mixer_hgrn2(h, hc, hg_w_in[j], hg_w_out[j], lb_all[l], hg_norm[j],
                                need_ctx=not last)
        x = x + g1 * y
        h = rmsnorm(x, norm_ffn[l]) * (1.0 + sc2) + sh2
        if last:
            ffn = grouped_moe(h.reshape(-1, d), router_w, router_bias,
                              moe_w_gate[l], moe_w_up[l], moe_w_down[l])
            x = x + g2 * ffn.reshape(x.shape)
        else:
            cx = cx + cg1 * yc
            hc = rmsnorm(cx, norm_ffn[l]) * (1.0 + csc2) + csh2
            tokens = jnp.concatenate([h.reshape(-1, d), hc.reshape(-1, d)], axis=0)
            ffn = grouped_moe(tokens, router_w, router_bias,
                              moe_w_gate[l], moe_w_up[l], moe_w_down[l])
            n_lat = x.shape[0] * x.shape[1]
            x = x + g2 * ffn[:n_lat].reshape(x.shape)
            cx = cx + cg2 * ffn[n_lat:].reshape(cx.shape)
    return rmsnorm(x, final_norm)
```

```python
import contextlib
import math
import numpy as np
import concourse.bass as bass
import concourse.mybir as mybir
from concourse.bass_utils import run_bass_kernel_spmd

F32 = mybir.dt.float32
BF16 = mybir.dt.bfloat16
I32 = mybir.dt.int32
ALU = mybir.AluOpType
AF = mybir.ActivationFunctionType
AX = mybir.AxisListType

COMPUTE = ("pe", "act", "dve", "pool")
N_DMA_SEMS = 12
D = 1024
NCTX = 256
NLAT = 4096
T = NCTX + NLAT
NT = T // 128
EPS = 1e-6


class Prog:
    def __init__(self, nc, es):
        self.nc = nc
        self.csem = {e: es.enter_context(nc.semaphore("s_" + e)) for e in COMPUTE}
        self.dsem = {q: [es.enter_context(nc.semaphore("d_%s%d" % (q, i))) for i in range(N_DMA_SEMS)]
                     for q in ("sp", "pool")}
        self.cnt = {e: 0 for e in COMPUTE}
        self.ndma = {"sp": 0, "pool": 0}
        self.begin()

    def begin(self):
        self.ops = []
        self.state = {}
        self.lastdma = {}

    def capture(self, f):
        self.cap = []
        f()
        out, self.cap = self.cap, None
        return out

    def replay(self, lst):
        for a in lst:
            self._add(*a)

    def merge(self, A, B):
        na, nb = len(A), len(B)
        ia = ib = 0
        while ia < na or ib < nb:
            if ib >= nb or (ia < na and ia * nb <= ib * na):
                self._add(*A[ia]); ia += 1
            else:
                self._add(*B[ib]); ib += 1

    def _add(self, eng, kind, fn, reads, writes, sreads=()):
        if getattr(self, "cap", None) is not None:
            self.cap.append((eng, kind, fn, tuple(reads), tuple(writes), tuple(sreads)))
            return None
        op = dict(eng=eng, kind=kind, fn=fn, deps=[], idx=len(self.ops), signal=False)
        deps = set()
        strict = set()
        reads = tuple(reads) + tuple(sreads)
        for k in reads:
            st = self.state.setdefault(k, [None, []])
            if st[0] is not None:
                deps.add(st[0]["idx"])
                if eng != "pe":
                    strict.add(st[0]["idx"])
        for k in tuple(writes) + tuple(kk for kk in reads if isinstance(kk, str) and kk.startswith("ps")):
            st = self.state.setdefault(k, [None, []])
            if st[0] is not None:
                deps.add(st[0]["idx"])
            for r in st[1]:
                deps.add(r["idx"])
        if kind == "dma":
            n = self.ndma[eng]
            self.ndma[eng] += 1
            op["dslot"] = n % N_DMA_SEMS
            op["dround"] = n // N_DMA_SEMS + 1
            prev = self.lastdma.get((eng, op["dslot"]))
            if prev is not None:
                deps.add(prev["idx"])
            self.lastdma[(eng, op["dslot"])] = op
        for d in sorted(deps):
            o = self.ops[d]
            if o["kind"] == "compute" and o["eng"] == eng and d not in strict:
                continue
            op["deps"].append(d)
            o["signal"] = True
        for k in reads:
            self.state[k][1].append(op)
        for k in writes:
            self.state[k] = [op, []]
        self.ops.append(op)
        return op

    def pe(self, fn, r=(), w=(), sr=()):
        return self._add("pe", "compute", fn, r, w, sr)

    def act(self, fn, r=(), w=(), sr=()):
        return self._add("act", "compute", fn, r, w, sr)

    def dve(self, fn, r=(), w=(), sr=()):
        return self._add("dve", "compute", fn, r, w, sr)

    def pool(self, fn, r=(), w=(), sr=()):
        return self._add("pool", "compute", fn, r, w, sr)

    def eng(self, name):
        return {"pe": self.pe, "act": self.act, "dve": self.dve, "pool": self.pool}[name]

    def dma(self, out, in_, r=(), w=(), q="sp", **kw):
        return self._add(q, "dma", lambda e: e.dma_start(out=out, in_=in_, **kw), r, w)

    def emit(self):
        nc = self.nc
        csem, dsem, cnt = self.csem, self.dsem, self.cnt
        for op in self.ops:
            if op["kind"] == "compute" and op["signal"]:
                cnt[op["eng"]] += 1
                op["count"] = cnt[op["eng"]]
        last_dma = dict(self.lastdma)
        with nc.Block() as block:
            engs = {"pe": block.tensor, "act": block.scalar, "dve": block.vector,
                    "pool": block.gpsimd, "sp": block.sync}
            for ename, deco in engs.items():
                myops = [o for o in self.ops if o["eng"] == ename]

                def body(e, myops=myops, ename=ename):
                    waited = {}
                    for op in myops:
                        for d in op["deps"]:
                            o = self.ops[d]
                            if o["kind"] == "compute":
                                key = ("c", o["eng"]); val = o["count"]; sem = csem[o["eng"]]
                            else:
                                key = ("d", o["eng"], o["dslot"]); val = 16 * o["dround"]
                                sem = dsem[o["eng"]][o["dslot"]]
                            if waited.get(key, 0) >= val:
                                continue
                            waited[key] = val
                            e.wait_ge(sem, val)
                        ins = op["fn"](e)
                        if op["kind"] == "dma":
                            ins.then_inc(dsem[op["eng"]][op["dslot"]], 16)
                        elif op["signal"]:
                            ins.then_inc(csem[op["eng"]], 1)
                    if ename == "sp":
                        for (q, s_), o in last_dma.items():
                            e.wait_ge(dsem[q][s_], 16 * o["dround"])
                deco(body)
        nc.all_engine_barrier()
        self.begin()


class K:
    pass


_UID = [0]


def _sb(nc, es):
    def f(name, shape, dt=F32):
        _UID[0] += 1
        return es.enter_context(nc.sbuf_tensor("sb%d_%s" % (_UID[0], name), list(shape), dt))
    return f


def mm(P, out, lhsT, rhs, start=True, stop=True, r=(), w=()):
    P.pe(lambda e: e.matmul(out, lhsT, rhs, start=start, stop=stop), r, w)


def tr(P, out, in_, ident, r=(), w=()):
    P.pe(lambda e: e.transpose(out, in_, ident), r, w)


def actf(P, out, in_, func, r=(), w=(), sr=(), **kw):
    P.act(lambda e: e.activation(out, in_, func, **kw), r, w, sr)


def tt(P, eng, out, in0, in1, op, r=(), w=()):
    P.eng(eng)(lambda e: e.tensor_tensor(out, in0, in1, op), r, w)


def ts(P, eng, out, in0, s1, s2, op0, op1=None, r=(), w=(), sr=()):
    if op1 is None:
        P.eng(eng)(lambda e: e.tensor_scalar(out, in0, s1, None, op0), r, w, sr)
    else:
        P.eng(eng)(lambda e: e.tensor_scalar(out, in0, s1, s2, op0, op1), r, w, sr)


def stt(P, eng, out, in0, scalar, in1, op0, op1, r=(), w=(), sr=()):
    P.eng(eng)(lambda e: e.scalar_tensor_tensor(out, in0, scalar, in1, op0, op1), r, w, sr)


def cp(P, eng, out, in_, r=(), w=()):
    if eng == "act":
        P.act(lambda e: e.copy(out, in_), r, w)
    else:
        P.eng(eng)(lambda e: e.tensor_copy(out, in_), r, w)


def red(P, eng, out, in_, op, r=(), w=()):
    P.eng(eng)(lambda e: e.tensor_reduce(out, in_, AX.X, op), r, w)


def bc(ap, shape):
    return ap.to_broadcast(list(shape))


def alloc_consts(k):
    sb = k.sbp
    k.identf = sb("identf", [128, 128])
    k.identb = sb("identb", [128, 128], BF16)
    k.maskf = sb("maskf", [128, 128])
    k.maskb = sb("maskb", [128, 128])
    k.scanm = sb("scanm", [128, 128])
    k.sel = sb("sel", [32, 32, 128], BF16)
    k.cols = sb("cols", [128, 2, 4, 8, 2])
    k.posB = sb("posB", [128, 512])


def stage_consts(k, sbt):
    nc, P = k.nc, k.P
    P.pool(lambda e: e.memset(k.identf[:], 1.0), w=["identf"])
    P.pool(lambda e: e.affine_select(k.identf[:], k.identf[:], [[-1, 128]], ALU.is_equal, 0.0, base=0,
                                     channel_multiplier=1), r=["identf"], w=["identf"])
    cp(P, "dve", k.identb[:], k.identf[:], r=["identf"], w=["identb"])
    for name, tile_, sgn in (("maskf", k.maskf, 1), ("maskb", k.maskb, -1)):
        P.pool(lambda e, t=tile_: e.memset(t[:], 1.0), w=[name])
        P.pool(lambda e, t=tile_, c=sgn: e.affine_select(t[:], t[:], [[c, 128]], ALU.is_ge, 0.0, base=0,
                                                        channel_multiplier=-c), r=[name], w=[name])
        P.pool(lambda e, t=tile_: e.memset(t[0:64, 64:128], 0.0), r=[name], w=[name])
        P.pool(lambda e, t=tile_: e.memset(t[64:128, 0:64], 0.0), r=[name], w=[name])
    P.pool(lambda e: e.memset(k.scanm[:], 1.0), w=["scanm"])
    P.pool(lambda e: e.memset(k.scanm[:, 0:1], 0.0), r=["scanm"], w=["scanm"])
    P.pool(lambda e: e.memset(k.scanm[:, 64:65], 0.0), r=["scanm"], w=["scanm"])
    k.self32 = sbt("self32", [32, 32, 128])
    P.pool(lambda e: e.memset(k.self32[:], 1.0), w=["self32"])
    P.pool(lambda e: e.affine_select(k.self32[:], k.self32[:], [[-1, 32], [0, 128]], ALU.is_equal, 0.0, base=0,
                                     channel_multiplier=1), r=["self32"], w=["self32"])
    cp(P, "dve", k.sel[:], k.self32[:], r=["self32"], w=["sel"])


def stage_prologue(k):
    nc, P = k.nc, k.P
    with contextlib.ExitStack() as es:
        sb = _sb(nc, es)
        stage_consts(k, sb)
        csv = sb("csv", [128, 8, 2]); sv = sb("sv", [128, 8, 2])
        nmix = sb("nmix", [128, 2, 8]); nffn = sb("nffn", [128, 2, 8])
        P.dma(csv[:], k.d["csv"], w=["csv"])
        P.dma(nmix[:], k.d["nmix"], w=["nmix"])
        P.dma(nffn[:], k.d["nffn"], w=["nffn"])
        actf(P, sv[:], csv[:], AF.Silu, r=["csv"], w=["sv"])
        aw = [sb("aw%d" % i, [128, 3072]) for i in range(2)]
        modrow = sb("modrow", [2, 6144]); adab = sb("adab", [2, 6144])
        modcol = sb("modcol", [128, 48, 2])
        ps = k.ps
        for l in range(2):
            P.dma(adab[:], k.d["ada_b"][l:l + 1, :].partition_broadcast(2), w=["adab"])
            it = 0
            for ch in range(2):
                for kc in range(8):
                    a = aw[it % 2]; akey = "aw%d" % (it % 2); it += 1
                    P.dma(a[:], k.d["ada_w"][l, kc * 128:(kc + 1) * 128, ch * 3072:(ch + 1) * 3072], w=[akey])
                    for j in range(6):
                        mm(P, ps[j][0:2, :], sv[:, kc, :], a[:, j * 512:(j + 1) * 512], start=(kc == 0), stop=(kc == 7),
                           r=["sv", akey], w=["ps%d" % j])
                for j in range(6):
                    c0 = (ch * 6 + j) * 512
                    tt(P, "dve", modrow[:, c0:c0 + 512], ps[j][0:2, :], adab[:, c0:c0 + 512], ALU.add,
                       r=["ps%d" % j, "adab"], w=["modrow"])
            P.dma(k.d["modd"][l], modrow[:], r=["modrow"], w=["modd"])
            pst = ps[6][:, 0:96].rearrange("p (c j) -> p c j", j=2)
            for c in range(48):
                tr(P, pst[:, c, :], modrow[0:2, c * 128:(c + 1) * 128], k.identf[0:2, 0:2], r=["modrow", "identf"], w=["ps6"])
            cp(P, "dve", modcol[:], pst, r=["ps6"], w=["modcol"])
            C = k.cols
            for (ai, bi, nrm, sc0, sh0) in ((0, 1, nmix, 8, 0), (2, 3, nffn, 32, 24)):
                ts(P, "dve", C[:, l, ai], modcol[:, sc0:sc0 + 8, :], 1.0, None, ALU.add, r=["modcol"], w=["cols"])
                tt(P, "dve", C[:, l, ai], C[:, l, ai], bc(nrm[:, l, :].unsqueeze(2), [128, 8, 2]), ALU.mult,
                   r=["cols", "nmix", "nffn"], w=["cols"])
                cp(P, "dve", C[:, l, bi], modcol[:, sh0:sh0 + 8, :], r=["modcol"], w=["cols"])
        io = sb("io", [128, 256], I32); om = sb("om", [128, 256]); pc_i = sb("pc_i", [128, 1], I32)
        pc = sb("pc", [128, 1]); pm = sb("pm", [128, 1]); ang = sb("ang", [128, 256])
        y = sb("yy", [128, 512]); yi = sb("yi", [128, 512], I32); yf = sb("yf", [128, 512])
        P.pool(lambda e: e.iota(io[:], [[1, 256]], base=0, channel_multiplier=0), w=["io"])
        P.pool(lambda e: e.iota(pc_i[:], [[0, 1]], base=0, channel_multiplier=1), w=["pc_i"])
        cp(P, "dve", om[:], io[:], r=["io"], w=["om"])
        P.dve(lambda e: e.tensor_single_scalar(pc_i[:], pc_i[:], 63, ALU.bitwise_and), r=["pc_i"], w=["pc_i"])
        cp(P, "pool", pc[:], pc_i[:], r=["pc_i"], w=["pc"])
        actf(P, om[:], om[:], AF.Exp, r=["om"], w=["om"], scale=float(-math.log(10000.0) / 256.0))
        ts(P, "dve", ang[:], om[:], pc[:, 0:1], None, ALU.mult, r=["om"], sr=["pc"], w=["ang"])
        ts(P, "dve", y[:, 0:256], ang[:], float(1 / (2 * math.pi)), 0.5, ALU.mult, ALU.add, r=["ang"], w=["yy"])
        ts(P, "dve", y[:, 256:512], ang[:], float(1 / (2 * math.pi)), 0.75, ALU.mult, ALU.add, r=["ang"], w=["yy"])
        sin_reduced(P, k.posB[:], y[:], yi[:], yf[:], "yy", "yi", "yf", "posB")
        P.dma(k.d["t64d"], k.posB[0:64, :], r=["posB"], w=["t64d"])
        P.emit()


def sin_reduced(P, out, y, yi, yf, ky, kyi, kyf, kout):
    cp(P, "dve", yi, y, r=[ky], w=[kyi])
    cp(P, "dve", yf, yi, r=[kyi], w=[kyf])
    tt(P, "dve", y, y, yf, ALU.subtract, r=[ky, kyf], w=[ky])
    P.dve(lambda e: e.tensor_single_scalar(yf, y, 0.0, ALU.is_lt), r=[ky], w=[kyf])
    tt(P, "dve", y, y, yf, ALU.add, r=[ky, kyf], w=[ky])
    actf(P, out, y, AF.Sin, r=[ky], w=[kout], bias=float(-math.pi), scale=float(2 * math.pi))


def norm_tile(k, xt, kx, A, Bc, hout, khout, sfx=""):
    P = k.P
    junk, ss = k.njunk, k.nss
    actf(P, junk[:], xt, AF.Square, r=[kx], w=["njunk"])
    red(P, "dve", ss[:], junk[:], ALU.add, r=["njunk"], w=["nss"])
    ts(P, "dve", ss[:], ss[:], 1.0 / D, EPS, ALU.mult, ALU.add, r=["nss"], w=["nss"])
    actf(P, ss[:], ss[:], AF.Sqrt, r=["nss"], w=["nss"])
    P.dve(lambda e: e.reciprocal(ss[:], ss[:]), r=["nss"], w=["nss"])
    ts(P, "dve", junk[:], xt, ss[:, 0:1], None, ALU.mult, r=[kx], sr=["nss"], w=["njunk"])
    pa = k.ps[0][:].rearrange("p (c t) -> p c t", t=128)
    pb = k.ps[1][:].rearrange("p (c t) -> p c t", t=128)
    for c in range(8):
        dst = pa[:, c, :] if c < 4 else pb[:, c - 4, :]
        tr(P, dst, junk[:, c * 128:(c + 1) * 128], k.identf[:], r=["njunk", "identf"], w=["ps%d" % (c // 4)])
    for half, pp in ((0, pa), (1, pb)):
        tt(P, "dve", hout[:, half * 4:half * 4 + 4, :], pp, bc(A[:, half * 4:half * 4 + 4].unsqueeze(2), [128, 4, 128]),
           ALU.mult, r=["ps%d" % half, "cols"], w=[khout])
        tt(P, "dve", hout[:, half * 4:half * 4 + 4, :], hout[:, half * 4:half * 4 + 4, :],
           bc(Bc[:, half * 4:half * 4 + 4].unsqueeze(2), [128, 4, 128]), ALU.add, r=[khout, "cols"], w=[khout])


def stage_A(k, l, es_ext=None):
    nc, P = k.nc, k.P
    with contextlib.ExitStack() as es_own:
        es = es_ext if es_ext is not None else es_own
        sb = _sb(nc, es)
        k.njunk = sb("njunk", [128, 1024]); k.nss = sb("nss", [128, 1])
        xt = [sb("xt%d" % i, [128, 1024]) for i in range(2)]
        pa = [sb("pa%d" % i, [128, 512]) for i in range(2)]
        h32 = [sb("h32_%d" % i, [128, 8, 128]) for i in range(2)]
        hb = [sb("hb%d" % i, [128, 8, 128], BF16) for i in range(2)]
        for i in range(NT):
            b = i % 2
            x, kx = xt[b], "xt%d" % b
            lat = i >= 2
            if l == 0:
                P.dma(x[:], k.d["xin"][i * 128:(i + 1) * 128, :], w=[kx])
                if lat:
                    r0 = (i - 2) * 2
                    P.dma(pa[b][0:64, :], k.d["t64d"][r0:r0 + 1, :].partition_broadcast(64), r=["t64d"], w=["pa%d" % b])
                    P.dma(pa[b][64:128, :], k.d["t64d"][r0 + 1:r0 + 2, :].partition_broadcast(64), r=["t64d"], w=["pa%d" % b])
                    tt(P, "pool", x[:, 0:512], x[:, 0:512], pa[b][:], ALU.add, r=[kx, "pa%d" % b], w=[kx])
                    tt(P, "pool", x[:, 512:1024], x[:, 512:1024], k.posB[:], ALU.add, r=[kx, "posB"], w=[kx])
                P.dma(k.d["X"][i * 128:(i + 1) * 128, :], x[:], r=[kx], w=["X%d" % i])
            else:
                P.dma(x[:], k.d["X"][i * 128:(i + 1) * 128, :], r=["X%d" % i], w=[kx])
            j = 0 if lat else 1
            norm_tile(k, x[:], kx, k.cols[:, l, 0, :, j], k.cols[:, l, 1, :, j], h32[b], "h32_%d" % b)
            cp(P, "act", hb[b][:], h32[b][:], r=["h32_%d" % b], w=["hb%d" % b])
            P.dma(k.d["hT"][:, :, i * 128:(i + 1) * 128], hb[b][:], r=["hb%d" % b], w=["hT%d" % i])
        if es_ext is None:
            P.emit()


def stage_precast(k, l, es_ext=None):
    nc, P = k.nc, k.P
    with contextlib.ExitStack() as es_own:
        es = es_ext if es_ext is not None else es_own
        sb = _sb(nc, es)
        st = [sb("st%d" % i, [128, 8, 512]) for i in range(3)]
        sd = [sb("sd%d" % i, [128, 2, 1024]) for i in range(3)]
        bt_ = [sb("bt%d" % i, [128, 8, 512], BF16) for i in range(3)]
        bd = [sb("bd%d" % i, [128, 2, 1024], BF16) for i in range(3)]
        engs = ("act", "dve", "pool")
        for e_ in range(32):
            b = e_ % 3
            P.dma(st[b][:, :, 0:256], k.d["moe_wg"][l, e_].rearrange("(c p) n -> p c n", p=128), w=["st%d" % b])
            P.dma(st[b][:, :, 256:512], k.d["moe_wu"][l, e_].rearrange("(c p) n -> p c n", p=128), w=["st%d" % b])
            P.dma(sd[b][:], k.d["moe_wd"][l, e_].rearrange("(c p) n -> p c n", p=128), w=["sd%d" % b])
            cp(P, engs[e_ % 3], bt_[b][:], st[b][:], r=["st%d" % b], w=["bt%d" % b])
            cp(P, engs[(e_ + 1) % 3], bd[b][:], sd[b][:], r=["sd%d" % b], w=["bd%d" % b])
            P.dma(k.d["wgu_bf"][l, e_], bt_[b][:], r=["bt%d" % b], w=["wgu_o%d" % e_])
            P.dma(k.d["wd_bf"][l, e_], bd[b][:], r=["bd%d" % b], w=["wd_o%d" % e_])
        slabs = []
        win_d = k.d["w_in0"] if l == 0 else k.d["hg_w_in"]
        wout_d = k.d["w_out0"] if l == 0 else k.d["hg_w_out"]
        ncol = 2080 if l == 0 else 5120
        for c0 in range(0, ncol, 512):
            slabs.append((win_d, k.d["win_bf%d" % l], c0, min(512, ncol - c0)))
        for c0 in range(0, 1024, 512):
            slabs.append((wout_d, k.d["wout_bf%d" % l], c0, 512))
        for si_, (src, dst, c0, w_) in enumerate(slabs):
            b = (32 + si_) % 3
            P.dma(st[b][:, :, 0:w_], src[:, c0:c0 + w_].rearrange("(c p) n -> p c n", p=128), w=["st%d" % b])
            cp(P, engs[si_ % 3], bt_[b][:, :, 0:w_], st[b][:, :, 0:w_], r=["st%d" % b], w=["bt%d" % b])
            P.dma(dst[:, :, c0:c0 + w_], bt_[b][:, :, 0:w_], r=["bt%d" % b], w=["wslab%d" % si_])
        if es_ext is None:
            P.emit()


def stage_AC(k, l):
    P = k.P
    with contextlib.ExitStack() as es:
        a = P.capture(lambda: stage_A(k, l, es))
        c = P.capture(lambda: stage_precast(k, l, es))
        P.merge(a, c)
        P.emit()


def stage_moe(k, l, final):
    nc, P = k.nc, k.P
    ps = k.ps
    tiles = list(range(NT)) if l == 0 else list(range(2, NT))
    blocks = []
    if l == 0:
        blocks.append([0, 1])
    for b0 in range(2, NT, 8):
        blocks.append(list(range(b0, b0 + 8)))
    with contextlib.ExitStack() as es:
        sb = _sb(nc, es)
        k.njunk = sb("njunk", [128, 1024]); k.nss = sb("nss", [128, 1])
        xt = [sb("xt%d" % i, [128, 1024]) for i in range(2)]
        h32 = [sb("h32_%d" % i, [128, 8, 128]) for i in range(2)]
        h2T = sb("h2T", [128, 8, 1024], BF16)
        acc = sb("acc", [128, 8, 1024])
        gT = sb("gT", [32, 1024], BF16)
        rw = sb("rw", [128, 8, 32]); rb = sb("rb", [128, 32])
        g2bc = sb("g2bc", [128, 2, 1024]); fnbc = sb("fnbc", [128, 1024])
        wgu = [sb("wgu%d" % i, [128, 8, 512], BF16) for i in range(3)]
        wd = [sb("wd%d" % i, [128, 2, 1024], BF16) for i in range(8)]
        G = [sb("G%d" % i, [128, 2, 1024], BF16) for i in range(8)]
        sg = [sb("sg%d" % i, [128, 2, 256]) for i in range(2)]
        gbs = [sb("gbs%d" % i, [128, 256]) for i in range(2)]
        lg = sb("lg", [128, 32]); aff = sb("aff", [128, 32]); selv = sb("selv", [128, 32])
        cmp4 = sb("cmp4", [128, 8, 4, 4]); cnt = sb("cnt", [128, 32]); m2 = sb("m2", [128, 32])
        gs = sb("gs", [128, 8]); gmx = sb("gmx", [128, 1]); gsel = sb("gsel", [128, 8])
        wv = sb("wv", [128, 32]); wsum = sb("wsum", [128, 1]); gates = sb("gates", [128, 32])
        gtb = sb("gtb", [32, 128], BF16)
        yo = [sb("yo%d" % i, [128, 1024]) for i in range(2)]
        P.dma(rw[:], k.d["router_w"], w=["rw"])
        P.dma(rb[:], k.d["router_b"].partition_broadcast(128), w=["rb"])
        P.dma(g2bc[:, 0, :], k.d["modd"][l, 0:1, 5120:6144].partition_broadcast(128), w=["g2bc"])
        P.dma(g2bc[:, 1, :], k.d["modd"][l, 1:2, 5120:6144].partition_broadcast(128), w=["g2bc"])
        if final:
            P.dma(fnbc[:], k.d["fnorm"].partition_broadcast(128), w=["fnbc"])
        wg_d, wu_d, wd_d = k.d["moe_wg"], k.d["moe_wu"], k.d["moe_wd"]
        wit = 0
        sit = 0
        for blk in blocks:
            nb = len(blk)
            ntok = nb * 128
            for ti, i in enumerate(blk):
                b = i % 2
                x, kx = xt[b], "xt%d" % b
                P.dma(x[:], k.d["X"][i * 128:(i + 1) * 128, :], r=["X%d" % i], w=[kx])
                j = 0 if i >= 2 else 1
                hh, kh = h32[b], "h32_%d" % b
                norm_tile(k, x[:], kx, k.cols[:, l, 2, :, j], k.cols[:, l, 3, :, j], hh, kh)
                cp(P, "act", h2T[:, :, ti * 128:(ti + 1) * 128], hh[:], r=[kh], w=["h2T"])
                for c in range(8):
                    mm(P, ps[2][:, 0:32], hh[:, c, :], rw[:, c, :], start=(c == 0), stop=(c == 7), r=[kh, "rw"], w=["ps2"])
                actf(P, aff[:], ps[2][:, 0:32], AF.Sigmoid, r=["ps2"], w=["aff"])
                tt(P, "dve", selv[:], aff[:], rb[:], ALU.add, r=["aff", "rb"], w=["selv"])
                s3 = selv[:].rearrange("p (g e) -> p g e", e=4)
                tt(P, "dve", cmp4[:], bc(s3.unsqueeze(2), [128, 8, 4, 4]), bc(s3.unsqueeze(3), [128, 8, 4, 4]), ALU.is_gt,
                   r=["selv"], w=["cmp4"])
                red(P, "dve", cnt[:], cmp4[:].rearrange("p g i j -> p (g i) j"), ALU.add, r=["cmp4"], w=["cnt"])
                P.dve(lambda e: e.tensor_single_scalar(m2[:], cnt[:], 1.5, ALU.is_lt), r=["cnt"], w=["m2"])
                tt(P, "dve", wv[:], selv[:], m2[:], ALU.mult, r=["selv", "m2"], w=["wv"])
                red(P, "dve", gs[:], wv[:].rearrange("p (g e) -> p g e", e=4), ALU.add, r=["wv"], w=["gs"])
                red(P, "dve", gmx[:], gs[:], ALU.max, r=["gs"], w=["gmx"])
                ts(P, "dve", gsel[:], gs[:], gmx[:, 0:1], None, ALU.is_ge, r=["gs"], sr=["gmx"], w=["gsel"])
                tt(P, "dve", m2[:].rearrange("p (g e) -> p g e", e=4), m2[:].rearrange("p (g e) -> p g e", e=4),
                   bc(gsel[:].unsqueeze(2), [128, 8, 4]), ALU.mult, r=["m2", "gsel"], w=["m2"])
                tt(P, "dve", wv[:], aff[:], m2[:], ALU.mult, r=["aff", "m2"], w=["wv"])
                red(P, "dve", wsum[:], wv[:], ALU.add, r=["wv"], w=["wsum"])
                P.dve(lambda e: e.reciprocal(wsum[:], wsum[:]), r=["wsum"], w=["wsum"])
                ts(P, "dve", gates[:], wv[:], wsum[:, 0:1], None, ALU.mult, r=["wv"], sr=["wsum"], w=["gates"])
                if k.debug and ("gates%d" % l) in k.d:
                    P.dma(k.d["gates%d" % l][i * 128:(i + 1) * 128, :], gates[:], r=["gates"])
                if k.debug and "dbgA" in k.d:
                    P.dma(k.d["dbgA"][i * 128:(i + 1) * 128, :], aff[:], r=["aff"])
                    P.dma(k.d["dbgB"][i * 128:(i + 1) * 128, :], selv[:], r=["selv"])
                    P.dma(k.d["dbgC"][i * 128:(i + 1) * 128, :], m2[:], r=["m2"])
                    P.dma(k.d["dbgD"][i * 128:(i + 1) * 128, :], gs[:], r=["gs"])
                    P.dma(k.d["dbgE"][i * 128:(i + 1) * 128, :], cnt[:], r=["cnt"])
                tr(P, ps[3][0:32, 0:128], gates[:], k.identf[:], r=["gates", "identf"], w=["ps3"])
                cp(P, "act", gT[:, ti * 128:(ti + 1) * 128], ps[3][0:32, 0:128], r=["ps3"], w=["gT"])
            subs = [(s0, min(256, ntok - s0)) for s0 in range(0, ntok, 256)]
            dsubs = [(s0, min(512, ntok - s0)) for s0 in range(0, ntok, 512)]
            for eg in range(8):
                for el in range(4):
                    e_ = eg * 4 + el
                    wb = wgu[wit % 3]; kwb = "wgu%d" % (wit % 3); wit += 1
                    slot = (eg % 2) * 4 + el
                    wdd, kwd = wd[slot], "wd%d" % slot
                    Gs, kG = G[slot], "G%d" % slot
                    P.dma(wb[:], k.d["wgu_bf"][l, e_], w=[kwb])
                    P.dma(wdd[:], k.d["wd_bf"][l, e_], w=[kwd])
                    for si, (s0, sn) in enumerate(subs):
                        par = sit % 2; sit += 1
                        sgb, ksg = sg[par], "sg%d" % par
                        gb_, kgb = gbs[par], "gbs%d" % par
                        gbp = ps[2][:, par * 256:par * 256 + sn]; kgbp = "ps2_%d" % par
                        gp = ps[3 + par][:].rearrange("p (c t) -> p c t", t=256); kgp = "ps%d" % (3 + par)
                        up = ps[5 + par][:].rearrange("p (c t) -> p c t", t=256); kup = "ps%d" % (5 + par)
                        mm(P, gbp, k.sel[:, e_, :], gT[:, s0:s0 + sn], r=["sel", "gT"], w=["ps2"])
                        for c2 in range(2):
                            for c in range(8):
                                mm(P, gp[:, c2, 0:sn], wb[:, c, c2 * 128:(c2 + 1) * 128], h2T[:, c, s0:s0 + sn],
                                   start=(c == 0), stop=(c == 7), r=[kwb, "h2T"], w=[kgp])
                        for c2 in range(2):
                            for c in range(8):
                                mm(P, up[:, c2, 0:sn], wb[:, c, 256 + c2 * 128:256 + (c2 + 1) * 128], h2T[:, c, s0:s0 + sn],
                                   start=(c == 0), stop=(c == 7), r=[kwb, "h2T"], w=[kup])
                        cp(P, "act", gb_[:, 0:sn], gbp, r=["ps2"], w=[kgb])
                        actf(P, sgb[:, :, 0:sn], gp[:, :, 0:sn], AF.Silu, r=[kgp], w=[ksg])
                        tt(P, "pool", sgb[:, :, 0:sn], sgb[:, :, 0:sn], bc(gb_[:, 0:sn].unsqueeze(1), [128, 2, sn]), ALU.mult,
                           r=[ksg, kgb], w=[ksg])
                        tt(P, "dve", Gs[:, :, s0:s0 + sn], up[:, :, 0:sn], sgb[:, :, 0:sn], ALU.mult, r=[kup, ksg], w=[kG])
                for si, (s0, sn) in enumerate(dsubs):
                    for dc in range(8):
                        pb_ = ps[dc % 2]; kpb = "ps%d" % (dc % 2)
                        n = 0
                        for el in range(4):
                            slot = (eg % 2) * 4 + el
                            for c2 in range(2):
                                mm(P, pb_[:, 0:sn], wd[slot][:, c2, dc * 128:(dc + 1) * 128], G[slot][:, c2, s0:s0 + sn],
                                   start=(n == 0), stop=(n == 7), r=["wd%d" % slot, "G%d" % slot], w=[kpb])
                                n += 1
                        if eg == 0:
                            cp(P, "dve", acc[:, dc, s0:s0 + sn], pb_[:, 0:sn], r=[kpb], w=["acc"])
                        else:
                            tt(P, "dve", acc[:, dc, s0:s0 + sn], acc[:, dc, s0:s0 + sn], pb_[:, 0:sn], ALU.add,
                               r=[kpb, "acc"], w=["acc"])
            for ti, i in enumerate(blk):
                b = i % 2
                x, kx = xt[b], "xt%d" % b
                y_, ky = yo[b], "yo%d" % b
                P.dma(x[:], k.d["X"][i * 128:(i + 1) * 128, :], r=["X%d" % i], w=[kx])
                for c in range(8):
                    pp = ps[2 + c // 4]
                    tr(P, pp[:, (c % 4) * 128:(c % 4 + 1) * 128], acc[:, c, ti * 128:(ti + 1) * 128], k.identf[:],
                       r=["acc", "identf"], w=["ps%d" % (2 + c // 4)])
                j = 0 if i >= 2 else 1
                if k.debug and ("ffn%d" % l) in k.d:
                    for hf in range(2):
                        cp(P, "act", y_[:, hf * 512:(hf + 1) * 512], ps[2 + hf][:], r=["ps%d" % (2 + hf)], w=[ky])
                    P.dma(k.d["ffn%d" % l][i * 128:(i + 1) * 128, :], y_[:], r=[ky])
                for hf in range(2):
                    tt(P, "dve", y_[:, hf * 512:(hf + 1) * 512], ps[2 + hf][:], g2bc[:, j, hf * 512:(hf + 1) * 512], ALU.mult,
                       r=["ps%d" % (2 + hf), "g2bc"], w=[ky])
                tt(P, "pool", x[:], x[:], y_[:], ALU.add, r=[kx, ky], w=[kx])
                if not final:
                    P.dma(k.d["X"][i * 128:(i + 1) * 128, :], x[:], r=[kx], w=["X%d" % i])
                else:
                    ss2 = k.nss
                    actf(P, y_[:], x[:], AF.Square, r=[kx], w=[ky])
                    red(P, "dve", ss2[:], y_[:], ALU.add, r=[ky], w=["nss"])
                    ts(P, "dve", ss2[:], ss2[:], 1.0 / D, EPS, ALU.mult, ALU.add, r=["nss"], w=["nss"])
                    actf(P, ss2[:], ss2[:], AF.Sqrt, r=["nss"], w=["nss"])
                    P.dve(lambda e: e.reciprocal(ss2[:], ss2[:]), r=["nss"], w=["nss"])
                    stt(P, "dve", y_[:], x[:], ss2[:, 0:1], fnbc[:], ALU.mult, ALU.mult, r=[kx, "fnbc"], sr=["nss"], w=[ky])
                    P.dma(k.d["out"][(i - 2) * 128:(i - 1) * 128, :], y_[:], r=[ky])
        P.emit()


INPUT_SPECS = {
    "xin": ([T, D], F32), "csv": ([128, 8, 2], F32), "ada_w": ([2, D, 6 * D], F32), "ada_b": ([2, 6 * D], F32),
    "nmix": ([128, 2, 8], F32), "nffn": ([128, 2, 8], F32), "fnorm": ([1, D], F32),
    "w_in0": ([D, 2080], F32), "w_out0": ([D, D], F32), "a2blk": ([2, 33, 256], F32), "gnorm0": ([1, 512], F32),
    "s5_lam": ([64, 2, 2, 32], F32), "s5_logdt": ([1, 64], F32), "s5_b": ([64, 2, 2, 32, 16], F32),
    "s5_c": ([64, 2, 2, 32, 16], F32), "s5_d": ([1, 512], F32), "glu_w": ([512, 512], F32), "glu_b": ([128, 4], F32),
    "hg_w_in": ([D, 5 * D], F32), "hg_w_out": ([D, D], F32), "hg_lbrow": ([2, D], F32), "hg_lbcol": ([128, 2, 8], F32),
    "gnorm1": ([1, 512], F32),
    "router_w": ([128, 8, 32], F32), "router_b": ([1, 32], F32),
    "moe_wg": ([2, 32, D, 256], F32), "moe_wu": ([2, 32, D, 256], F32), "moe_wd": ([2, 32, 256, D], F32),
}
SCRATCH_SPECS = {
    "modd": ([2, 2, 6 * D], F32), "t64d": ([64, 512], F32), "X": ([T, D], F32), "hT": ([128, 8, T], BF16),
    "of": ([T, D], F32), "s5oT": ([128, 4, T], BF16), "u_d": ([T, 512], F32),
    "wgu_bf": ([2, 32, 128, 8, 512], BF16), "wd_bf": ([2, 32, 128, 2, 1024], BF16),
    "win_bf0": ([128, 8, 2080], BF16), "wout_bf0": ([128, 8, 1024], BF16),
    "win_bf1": ([128, 8, 5120], BF16), "wout_bf1": ([128, 8, 1024], BF16),
}


def build(stages, debug=False, ext_in=(), ext_out=(), dbg=None):
    nc = bass.Bass("TRN2", target_bir_lowering=False)
    k = K()
    k.nc = nc
    k.debug = debug
    k.d = {}
    for name, (shape, dt) in INPUT_SPECS.items():
        k.d[name] = nc.dram_tensor(name, shape, dt, kind="ExternalInput").ap()
    for name, (shape, dt) in SCRATCH_SPECS.items():
        kind = "ExternalInput" if name in ext_in else ("ExternalOutput" if name in ext_out else "Internal")
        k.d[name] = nc.dram_tensor(name, shape, dt, kind=kind).ap()
    for name, (shape, dt) in (dbg or {}).items():
        k.d[name] = nc.dram_tensor(name, shape, dt, kind="ExternalOutput").ap()
    k.d["out"] = nc.dram_tensor("out", [NLAT, D], F32, kind="ExternalOutput").ap()
    with contextlib.ExitStack() as es0:
        k.P = Prog(nc, es0)
        k.sbp = _sb(nc, es0)
        k.ps = [es0.enter_context(nc.psum_tensor("ps%d" % i, [128, 512], F32)) for i in range(7)]
        k.psb = es0.enter_context(nc.psum_tensor("psb", [128, 1024], BF16))
        alloc_consts(k)
        for st in stages:
            if st == "pro":
                stage_prologue(k)
            elif st == "consts_only":
                k.P.emit()
            elif st[0] == "A":
                stage_A(k, int(st[1]))
            elif st[0] == "B":
                stage_AC(k, int(st[1]))
            elif st[0] == "M":
                stage_moe(k, int(st[1]), final=(st[1] == "1"))
            elif st == "S5":
                stage_s5(k)
            elif st[0] == "C":
                stage_precast(k, int(st[1]))
            elif st[0] == "R":
                stage_rec(k, int(st[1]))
            else:
                raise ValueError(st)
    return nc


def host_inputs(inp, b):
    f = lambda a: np.ascontiguousarray(np.asarray(a, dtype=np.float32))
    col = lambda v: f(np.asarray(v).reshape(8, 128).T)
    m = {}
    m["xin"] = f(np.concatenate([inp["ctx"][b], inp["x"][b]], axis=0))
    m["csv"] = f(np.stack([col(inp["c"][b]), col(inp["c_ctx"])], axis=-1))
    m["ada_w"] = f(inp["ada_w"]); m["ada_b"] = f(inp["ada_b"])
    m["nmix"] = f(np.stack([col(inp["norm_mix"][l]) for l in range(2)], axis=1))
    m["nffn"] = f(np.stack([col(inp["norm_ffn"][l]) for l in range(2)], axis=1))
    m["fnorm"] = f(inp["final_norm"].reshape(1, D))
    m["w_in0"] = f(inp["ab_w_in"][0]); m["w_out0"] = f(inp["ab_w_out"][0])
    a2 = np.zeros((2, 33, 256), np.float32)
    a2[0, 0:16] = inp["gla_a2"][0, 0]; a2[1, 16:32] = inp["gla_a2"][0, 1]
    a2[0, 32] = inp["gla_ab"][0, 0]; a2[1, 32] = inp["gla_ab"][0, 1]
    m["a2blk"] = a2
    m["gnorm0"] = f(np.tile(inp["gla_norm"][0], 4).reshape(1, 512))
    m["s5_lam"] = f(np.stack([inp["s5_lam_re"][0], inp["s5_lam_im"][0]], 0).transpose(3, 0, 1, 2))
    m["s5_logdt"] = f(inp["s5_log_dt"][0].reshape(1, 64))
    m["s5_b"] = f(np.stack([inp["s5_b_re"][0], inp["s5_b_im"][0]], 0).transpose(3, 0, 1, 2, 4))
    m["s5_c"] = f(np.stack([inp["s5_c_re"][0], inp["s5_c_im"][0]], 0).transpose(4, 0, 1, 2, 3))
    m["s5_d"] = f(inp["s5_d"][0].reshape(1, 512))
    m["glu_w"] = f(inp["s5_glu_w"][0]); m["glu_b"] = f(inp["s5_glu_b"][0].reshape(4, 128).T)
    m["hg_w_in"] = f(inp["hg_w_in"][0]); m["hg_w_out"] = f(inp["hg_w_out"][0])
    m["hg_lbrow"] = f(inp["hg_lb_logits"]); m["hg_lbcol"] = f(np.stack([col(inp["hg_lb_logits"][l]) for l in range(2)], axis=1))
    m["gnorm1"] = f(np.tile(inp["hg_norm"][0], 4).reshape(1, 512))
    m["router_w"] = f(inp["router_w"].reshape(8, 128, 32).transpose(1, 0, 2))
    m["router_b"] = f(inp["router_bias"].reshape(1, 32))
    m["moe_wg"] = f(inp["moe_w_gate"]); m["moe_wu"] = f(inp["moe_w_up"]); m["moe_wd"] = f(inp["moe_w_down"])
    return m


ALL_STAGES = ["pro", "B0", "S5", "R0", "M0", "B1", "R1", "M1"]


def kernel(**inputs):
    nc = build(ALL_STAGES)
    shared = None
    in_maps = []
    for b in range(8):
        m = host_inputs(inputs, b)
        if shared is None:
            shared = m
        else:
            for kk in m:
                if kk not in ("xin", "csv"):
                    m[kk] = shared[kk]
        in_maps.append(m)
    res = run_bass_kernel_spmd(nc, in_maps, core_ids=list(range(8)))
    return np.stack([np.asarray(r["out"], dtype=np.float32) for r in res.results], axis=0)


def stage_rec(k, l):
    nc, P, ps, psb = k.nc, k.P, k.ps, k.psb
    gla = (l == 0)
    NG = 1 if gla else 2
    J = 2 if gla else 4
    dk = 64 if gla else 128
    NW = 1568 if gla else 5120
    heads = []
    for h in range(4):
        heads.append((h // 2, (h % 2) * 64) if gla else (h, 0))
    win_d = k.d["w_in0"] if gla else k.d["hg_w_in"]
    wout_d = k.d["w_out0"] if gla else k.d["hg_w_out"]
    for di in range(2):
        with contextlib.ExitStack() as es:
            sb = _sb(nc, es)
            W = sb("W", [128, 8, NW], BF16)
            P.dma(W[:], k.d["win_bf%d" % l][:, :, 0:NW], w=["W"])
            hT = [sb("hT%d" % i, [128, 8, 128], BF16) for i in range(2)]
            S = sb("S", [128, NG, J, 128]); Sbf = sb("Sbf", [128, NG, J, 128], BF16)
            P.pool(lambda e: e.memset(S[:], 0.0), w=["S"])
            P.pool(lambda e: e.memset(Sbf[:], 0.0), w=["Sbf"])
            qT = sb("qT", [128, J, 128]); kT = sb("kT", [128, J, 128]); la = sb("la", [128, J, 128])
            Bp = sb("Bp", [128, J, 128]); Bt = sb("Bt", [128, J, 128]); D1 = sb("D1", [128, J, 128]); D2 = sb("D2", [128, J, 128])
            eq = sb("eq", [128, J, 128]); ek = sb("ek", [128, J, 128]); ei = sb("ei", [128, J, 128]); ekd = sb("ekd", [128, J, 128])
            dec = sb("dec", [128, J, 2])
            qs = sb("qs", [128, J, 128], BF16); ks_ = sb("ks", [128, J, 128], BF16); qi = sb("qi", [128, J, 128], BF16)
            kdT = sb("kdT", [128, J, 128], BF16); kd = sb("kd", [128, J * 128], BF16)
            vbf = sb("vbf", [128, 512], BF16); attn = sb("attn", [128, 4, 128], BF16)
            osb = [sb("osb%d" % i, [128, 512]) for i in range(2)]
            if gla:
                hm2 = sb("hm2", [128, 2]); qsm = sb("qsm", [128, 4, 128], BF16); qim = sb("qim", [128, 4, 128], BF16)
                P.pool(lambda e: e.memset(hm2[:], 0.0), w=["hm2"])
                P.pool(lambda e: e.memset(hm2[0:64, 0:1], 1.0), r=["hm2"], w=["hm2"])
                P.pool(lambda e: e.memset(hm2[64:128, 1:2], 1.0), r=["hm2"], w=["hm2"])
                a2 = sb("a2", [33, 256]); afbT = sb("afbT", [33, 128])
                P.dma(a2[:], k.d["a2blk"][di], w=["a2"])
                P.pool(lambda e: e.memset(afbT[32:33, :], 1.0), w=["afbT"])
            else:
                lbc = sb("lbc", [128, 2, 8]); lb = sb("lb", [128, 8]); oml = sb("oml", [128, 8]); sgm = sb("sgm", [128, J, 128])
                P.dma(lbc[:], k.d["hg_lbcol"], w=["lbc"])
                tt(P, "dve", lb[:], lbc[:, 1, :], lbc[:, 0, :], ALU.subtract, r=["lbc"], w=["lb"])
                actf(P, lb[:], lb[:], AF.Sigmoid, r=["lb"], w=["lb"])
                ts(P, "dve", oml[:], lb[:], -1.0, 1.0, ALU.mult, ALU.add, r=["lb"], w=["oml"])
            if di == 1:
                wout = sb("wout", [128, 8, 1024], BF16)
                P.dma(wout[:], k.d["wout_bf%d" % l], w=["wout"])
                gn = sb("gn", [128, 512]); g1bc = sb("g1bc", [128, 2, 1024])
                P.dma(gn[:], k.d["gnorm%d" % l].partition_broadcast(128), w=["gn"])
                P.dma(g1bc[:, 0, :], k.d["modd"][l, 0:1, 2048:3072].partition_broadcast(128), w=["g1bc"])
                P.dma(g1bc[:, 1, :], k.d["modd"][l, 1:2, 2048:3072].partition_broadcast(128), w=["g1bc"])
                oft = [sb("oft%d" % i, [128, 512]) for i in range(2)]
                o32 = sb("o32", [128, 4, 128]); sq = sb("sq", [128, 4, 128]); ssq = sb("ssq", [128, 4])
                sgl = sb("sgl", [128, 512]); mix = sb("mix", [128, 512], BF16)
                mixT = [sb("mixT%d" % i, [128, 8, 128], BF16) for i in range(2)]
                xt = [sb("xt%d" % i, [128, 1024]) for i in range(2)]
                yt = sb("yt", [128, 1024])
            order = list(range(NT)) if di == 0 else [1, 0] + list(range(NT - 1, 1, -1))
            mask = k.maskf if di == 0 else k.maskb
            kmask = "maskf" if di == 0 else "maskb"
            mid = 31 if di == 0 else 32
            end = 63 if di == 0 else 0
            v4 = lambda ap: ap.rearrange("p j (c t) -> p j c t", t=64)
            qiH = [sb("qiH%d" % i, [128, 4, 128], BF16) for i in range(2)]
            kdH = [sb("kdH%d" % i, [128, J * 128], BF16) for i in range(2)]
            vH = [sb("vH%d" % i, [128, 512], BF16) for i in range(2)]
            atH = [sb("atH%d" % i, [128, 4, 128], BF16) for i in range(2)]
            dcH = [sb("dcH%d" % i, [128, J, 2]) for i in range(2)]
            items = [(it, i, gi) for it, i in enumerate(order) for gi in range(NG)]
            p0 = ps[0][:].rearrange("p (j t) -> p j t", t=128)
            p1 = ps[1][:].rearrange("p (j t) -> p j t", t=128)
            p2 = ps[2][:].rearrange("p (j t) -> p j t", t=128)
            a_ps = ps[4][:].rearrange("p (h t) -> p h t", t=128)
            o_ps = ps[5][:].rearrange("p (h t) -> p h t", t=128)
            d_ps = ps[6][:].rearrange("p (j t) -> p j t", t=128)

            def front(n):
                it, i, gi = items[n]
                b = it % 2
                pn = n % 2
                h_, kh = hT[b], "hT%d" % b
                off = gi * 512
                if gi == 0:
                    P.dma(h_[:], k.d["hT"][:, :, i * 128:(i + 1) * 128], w=[kh])
                if gla:
                    for cc in range(4):
                        for c in range(8):
                            mm(P, p0[:, cc, :], W[:, c, cc * 128:(cc + 1) * 128], h_[:, c, :], start=(c == 0), stop=(c == 7),
                               r=["W", kh], w=["ps0"])
                    for c in range(8):
                        mm(P, ps[1][0:32, 0:128], W[:, c, 1536:1568], h_[:, c, :], start=(c == 0), stop=(c == 7),
                           r=["W", kh], w=["ps1"])
                    P.act(lambda e: e.mul(qT[:], p0[:, 0:2, :], 0.125), r=["ps0"], w=["qT"])
                    cp(P, "act", kT[:], p0[:, 2:4, :], r=["ps0"], w=["kT"])
                    cp(P, "dve", afbT[0:32, :], ps[1][0:32, 0:128], r=["ps1"], w=["afbT"])
                    for j in range(2):
                        mm(P, p2[:, j, :], a2[:, j * 128:(j + 1) * 128], afbT[:], r=["a2", "afbT"], w=["ps2"])
                    actf(P, la[:], p2[:, 0:2, :], AF.Exp, r=["ps2"], w=["la"], scale=-1.0)
                    actf(P, la[:], la[:], AF.Ln, r=["la"], w=["la"], bias=1.0)
                    ts(P, "dve", la[:], la[:], -1.0 / 16.0, None, ALU.mult, r=["la"], w=["la"])
                    vcol = 512
                else:
                    fcol = 1024 if di == 0 else 2048
                    for j in range(4):
                        for c in range(8):
                            mm(P, p0[:, j, :], W[:, c, off + j * 128: off + (j + 1) * 128], h_[:, c, :], start=(c == 0), stop=(c == 7),
                               r=["W", kh], w=["ps0"])
                    for j in range(4):
                        for c in range(8):
                            mm(P, p1[:, j, :], W[:, c, fcol + off + j * 128: fcol + off + (j + 1) * 128], h_[:, c, :],
                               start=(c == 0), stop=(c == 7), r=["W", kh], w=["ps1"])
                    actf(P, qT[:], p0, AF.Silu, r=["ps0"], w=["qT"])
                    actf(P, sgm[:], p1, AF.Sigmoid, r=["ps1"], w=["sgm"])
                    tt(P, "dve", sgm[:], sgm[:], bc(oml[:, gi * 4:gi * 4 + 4].unsqueeze(2), [128, 4, 128]), ALU.mult,
                       r=["sgm", "oml"], w=["sgm"])
                    tt(P, "dve", sgm[:], sgm[:], bc(lb[:, gi * 4:gi * 4 + 4].unsqueeze(2), [128, 4, 128]), ALU.add,
                       r=["sgm", "lb"], w=["sgm"])
                    ts(P, "dve", kT[:], sgm[:], -1.0, 1.0, ALU.mult, ALU.add, r=["sgm"], w=["kT"])
                    actf(P, la[:], sgm[:], AF.Ln, r=["sgm"], w=["la"])
                    vcol = 3072 + off
                vbf, kv = vH[pn], "vH%d" % pn
                for c in range(8):
                    mm(P, ps[3][:], h_[:, c, :], W[:, c, vcol:vcol + 512], start=(c == 0), stop=(c == 7), r=["W", kh], w=["ps3"])
                cp(P, "act", vbf[:], ps[3][:], r=["ps3"], w=[kv])
                for j in range(J):
                    P.dve(lambda e, j=j: e.tensor_tensor_scan(Bp[:, j, :], k.scanm[:], la[:, j, :], 0.0, ALU.mult, ALU.add),
                          r=["scanm", "la"], w=["Bp"])
                if di == 0:
                    Bq, kB = Bp, "Bp"
                else:
                    tt(P, "dve", Bt[:], la[:], Bp[:], ALU.subtract, r=["la", "Bp"], w=["Bt"])
                    tt(P, "dve", v4(Bt[:]), v4(Bt[:]), bc(v4(Bp[:])[:, :, :, 63:64], [128, J, 2, 64]), ALU.add, r=["Bt", "Bp"], w=["Bt"])
                    Bq, kB = Bt, "Bt"
                tt(P, "dve", v4(D1[:]), v4(Bq[:]), bc(v4(Bq[:])[:, :, :, mid:mid + 1], [128, J, 2, 64]), ALU.subtract, r=[kB], w=["D1"])
                tt(P, "pool", v4(D2[:]), bc(v4(Bq[:])[:, :, :, end:end + 1], [128, J, 2, 64]), v4(Bq[:]), ALU.subtract, r=[kB], w=["D2"])
                actf(P, eq[:], D1[:], AF.Exp, r=["D1"], w=["eq"])
                actf(P, ek[:], D1[:], AF.Exp, r=["D1"], w=["ek"], scale=-1.0)
                actf(P, ei[:], Bq[:], AF.Exp, r=[kB], w=["ei"])
                actf(P, ekd[:], D2[:], AF.Exp, r=["D2"], w=["ekd"])
                actf(P, dcH[pn][:], v4(Bq[:])[:, :, :, end], AF.Exp, r=[kB], w=["dcH%d" % pn])
                tt(P, "dve", qs[:], qT[:], eq[:], ALU.mult, r=["qT", "eq"], w=["qs"])
                tt(P, "pool", ks_[:], kT[:], ek[:], ALU.mult, r=["kT", "ek"], w=["ks"])
                tt(P, "dve", qi[:], qT[:], ei[:], ALU.mult, r=["qT", "ei"], w=["qi"])
                tt(P, "pool", kdT[:], kT[:], ekd[:], ALU.mult, r=["kT", "ekd"], w=["kdT"])
                for j in range(J):
                    tr(P, psb[:, j * 128:(j + 1) * 128], kdT[:, j, :], k.identb[:], r=["kdT", "identb"], w=["psb"])
                cp(P, "act", kdH[pn][:], psb[:, 0:J * 128], r=["psb"], w=["kdH%d" % pn])
                if gla:
                    hmb = bc(hm2[:].unsqueeze(1).unsqueeze(3), [128, 2, 2, 128])
                    tt(P, "pool", qsm[:].rearrange("p (j f) t -> p j f t", f=2), bc(qs[:].unsqueeze(2), [128, 2, 2, 128]), hmb, ALU.mult,
                       r=["qs", "hm2"], w=["qsm"])
                    tt(P, "pool", qiH[pn][:].rearrange("p (j f) t -> p j f t", f=2), bc(qi[:].unsqueeze(2), [128, 2, 2, 128]), hmb, ALU.mult,
                       r=["qi", "hm2"], w=["qiH%d" % pn])
                    qs_h, kqs = qsm, "qsm"
                else:
                    cp(P, "pool", qiH[pn][:], qi[:], r=["qi"], w=["qiH%d" % pn])
                    qs_h, kqs = qs, "qs"
                for h, (j, pb) in enumerate(heads):
                    mm(P, a_ps[:, h, :], ks_[:, j, :], qs_h[:, h, :], r=["ks", kqs], w=["ps4"])
                tt(P, "dve", atH[pn][:], a_ps, bc(mask[:].unsqueeze(1), [128, 4, 128]), ALU.mult, r=["ps4", kmask], w=["atH%d" % pn])

            def chain(n):
                it, i, gi = items[n]
                b = it % 2
                pn = n % 2
                h_, kh = hT[b], "hT%d" % b
                off = gi * 512
                lat = i >= 2
                need_out = gla or lat
                vbf, kv = vH[pn], "vH%d" % pn
                attn, kat = atH[pn], "atH%d" % pn
                kd, kkd = kdH[pn], "kdH%d" % pn
                qi_h, kqi = qiH[pn], "qiH%d" % pn
                dec, kdec = dcH[pn], "dcH%d" % pn
                gcol = 1024 if gla else 4096 + off
                for h in range(4):
                    mm(P, o_ps[:, h, :], attn[:, h, :], vbf[:, h * 128:(h + 1) * 128], start=(h == 0), stop=False,
                       r=[kat, kv], w=["ps5"])
                for ci, r0 in enumerate((0, 64) if di == 0 else (64, 0)):
                    cidx = r0 // 64
                    for h, (j, pb) in enumerate(heads):
                        mm(P, o_ps[r0:r0 + 64, h, :], qi_h[:, h, r0:r0 + 64], Sbf[:, gi, j, :], start=False, stop=True,
                           r=[kqi, "Sbf"], w=["ps5"])
                    for h, (j, pb) in enumerate(heads):
                        mm(P, d_ps[pb:pb + dk, j, :], kd[r0:r0 + 64, j * 128 + pb:j * 128 + pb + dk], vbf[r0:r0 + 64, h * 128:(h + 1) * 128],
                           r=[kkd, kv], w=["ps6"])
                    tt(P, "dve", S[:, gi], S[:, gi], bc(dec[:, :, cidx:cidx + 1], [128, J, 128]), ALU.mult, r=["S", kdec], w=["S"])
                    tt(P, "dve", S[:, gi], S[:, gi], d_ps[:, 0:J, :], ALU.add, r=["S", "ps6"], w=["S"])
                    cp(P, "act", Sbf[:, gi], S[:, gi], r=["S"], w=["Sbf"])
                if not need_out:
                    return
                if di == 0:
                    ob, kob = osb[n % 2], "osb%d" % (n % 2)
                    cp(P, "act", ob[:], ps[5][:], r=["ps5"], w=[kob])
                    P.dma(k.d["of"][i * 128:(i + 1) * 128, off:off + 512], ob[:], r=[kob], w=["of%d_%d" % (i, gi)])
                    return
                ofb, kofb = oft[n % 2], "oft%d" % (n % 2)
                P.dma(ofb[:], k.d["of"][i * 128:(i + 1) * 128, off:off + 512], w=[kofb])
                o3 = o32[:].rearrange("p h t -> p (h t)")
                tt(P, "dve", o3, ps[5][:], ofb[:], ALU.add, r=["ps5", kofb], w=["o32"])
                if k.debug and ("dbg_o%d" % l) in k.d:
                    P.dma(k.d["dbg_o%d" % l][i * 128:(i + 1) * 128, off:off + 512], o3, r=["o32"])
                tt(P, "pool", sq[:], o32[:], o32[:], ALU.mult, r=["o32"], w=["sq"])
                red(P, "dve", ssq[:], sq[:], ALU.add, r=["sq"], w=["ssq"])
                ts(P, "dve", ssq[:], ssq[:], 1.0 / 128, EPS, ALU.mult, ALU.add, r=["ssq"], w=["ssq"])
                actf(P, ssq[:], ssq[:], AF.Sqrt, r=["ssq"], w=["ssq"])
                P.dve(lambda e: e.reciprocal(ssq[:], ssq[:]), r=["ssq"], w=["ssq"])
                tt(P, "dve", o32[:], o32[:], bc(ssq[:].unsqueeze(2), [128, 4, 128]), ALU.mult, r=["o32", "ssq"], w=["o32"])
                tt(P, "pool", o3, o3, gn[:], ALU.mult, r=["o32", "gn"], w=["o32"])
                for c in range(8):
                    mm(P, ps[6][:], h_[:, c, :], W[:, c, gcol:gcol + 512], start=(c == 0), stop=(c == 7), r=["W", kh], w=["ps6"])
                actf(P, sgl[:], ps[6][:], AF.Silu, r=["ps6"], w=["sgl"])
                tt(P, "dve", mix[:], o3, sgl[:], ALU.mult, r=["o32", "sgl"], w=["mix"])
                mT, kmT = mixT[b], "mixT%d" % b
                for c in range(4):
                    tr(P, psb[:, 512 + c * 128:512 + (c + 1) * 128], mix[:, c * 128:(c + 1) * 128], k.identb[:], r=["mix", "identb"], w=["psb"])
                cp(P, "act", mT[:, gi * 4:gi * 4 + 4, :], psb[:, 512:1024].rearrange("p (c t) -> p c t", t=128), r=["psb"], w=[kmT])
                if gi != NG - 1:
                    return
                if gla:
                    P.dma(mT[:, 4:8, :], k.d["s5oT"][:, :, i * 128:(i + 1) * 128], w=[kmT])
                x, kx = xt[b], "xt%d" % b
                P.dma(x[:], k.d["X"][i * 128:(i + 1) * 128, :], w=[kx])
                for hf in range(2):
                    for c in range(8):
                        mm(P, ps[5 + hf][:], mT[:, c, :], wout[:, c, hf * 512:(hf + 1) * 512], start=(c == 0), stop=(c == 7),
                           r=[kmT, "wout"], w=["ps%d" % (5 + hf)])
                jj = 0 if lat else 1
                for hf in range(2):
                    tt(P, "dve", yt[:, hf * 512:(hf + 1) * 512], ps[5 + hf][:], g1bc[:, jj, hf * 512:(hf + 1) * 512], ALU.mult,
                       r=["ps%d" % (5 + hf), "g1bc"], w=["yt"])
                tt(P, "pool", x[:], x[:], yt[:], ALU.add, r=[kx, "yt"], w=[kx])
                P.dma(k.d["X"][i * 128:(i + 1) * 128, :], x[:], r=[kx], w=["Xw%d" % i])

            prevC = None
            for n in range(len(items)):
                fa = P.capture(lambda: front(n))
                if prevC is None:
                    P.replay(fa)
                else:
                    P.merge(fa, prevC)
                prevC = P.capture(lambda: chain(n))
            P.replay(prevC)
            P.emit()


def stage_s5(k):
    nc, P, ps, psb = k.nc, k.P, k.ps, k.psb
    CT = [(256, 128, 0), (256 + 2048, 128, 128), (0, 16, 256)]
    TWO_PI = 2.0 * math.pi
    with contextlib.ExitStack() as es:
        sb = _sb(nc, es)
        Wu = sb("Wu", [128, 8, 512], BF16)
        P.dma(Wu[:], k.d["win_bf0"][:, :, 1568:2080], w=["Wu"])
        Uf = sb("Uf", [128, 3, 16, 128])
        Ug = sb("Ug", [128, 8, 2, 272], BF16)
        Yp = sb("Yp", [128, 3, 16, 128], BF16)
        nrow = sb("nrow", [64, 5, 16]); nio = sb("nio", [64, 16], I32)
        P.pool(lambda e: e.iota(nio[:], [[1, 16]], base=0, channel_multiplier=0), w=["nio"])
        cp(P, "dve", nrow[:, 0, :], nio[:], r=["nio"], w=["nrow"])
        ts(P, "dve", nrow[:, 1, :], nrow[:, 0, :], -1.0, None, ALU.mult, r=["nrow"], w=["nrow"])
        ts(P, "dve", nrow[:, 2, :], nrow[:, 0, :], 1.0, None, ALU.add, r=["nrow"], w=["nrow"])
        ts(P, "dve", nrow[:, 3, :], nrow[:, 0, :], -1.0, 15.0, ALU.mult, ALU.add, r=["nrow"], w=["nrow"])
        ts(P, "dve", nrow[:, 4, :], nrow[:, 0, :], -1.0, 16.0, ALU.mult, ALU.add, r=["nrow"], w=["nrow"])
        tio = sb("tio", [128, 256], I32); tf = sb("tf", [128, 256]); pio = sb("pio", [128, 1], I32); slo = sb("slo", [128, 1])
        mT = sb("mT", [128, 2, 2, 256]); mtmp = sb("mtmp", [128, 256])
        P.pool(lambda e: e.iota(tio[:].rearrange("p (t h) -> p t h", h=16), [[1, 16], [0, 16]], base=0, channel_multiplier=0), w=["tio"])
        P.pool(lambda e: e.iota(pio[:], [[0, 1]], base=0, channel_multiplier=1), w=["pio"])
        cp(P, "dve", tf[:], tio[:], r=["tio"], w=["tf"])
        P.dve(lambda e: e.tensor_single_scalar(pio[:], pio[:], 4, ALU.arith_shift_right), r=["pio"], w=["pio"])
        cp(P, "pool", slo[:], pio[:], r=["pio"], w=["slo"])
        for sh in range(2):
            ts(P, "dve", mT[:, 0, sh, :], tf[:], float(-8 * sh), slo[:, 0:1], ALU.add, ALU.is_ge, r=["tf"], sr=["slo"], w=["mT"])
            ts(P, "dve", mtmp[:], tf[:], -1.0, float(8 * sh), ALU.mult, ALU.add, r=["tf"], w=["mtmp"])
            ts(P, "dve", mT[:, 1, sh, :], mtmp[:], slo[:, 0:1], 0.0, ALU.add, ALU.is_ge, r=["mtmp"], sr=["slo"], w=["mT"])
        dbc = sb("dbc", [128, 512])
        P.dma(dbc[:], k.d["s5_d"].partition_broadcast(128), w=["dbc"])
        lam = sb("lam", [64, 2, 2, 8]); ctl = sb("ctl", [64, 2, 2, 8, 16])
        Tre = sb("Tre", [64, 16, 5, 16]); Tim = sb("Tim", [64, 16, 5, 16])
        Bre = sb("Bre", [64, 16, 16]); Bim = sb("Bim", [64, 16, 16])
        Ar2 = sb("Ar2", [64, 2, 2, 8]); AiS = sb("AiS", [64, 2, 2, 8])
        P.emit()
        for fc in range(4):
            esA = contextlib.ExitStack(); sbA = _sb(nc, esA)
            hTc = sbA("hTc", [128, 8, 2048], BF16); Ub = sbA("Ub", [128, 3, 8, 16, 16], BF16)
            for ct, (tb, M, col0) in enumerate(CT):
                P.dma(hTc[:, :, 0:M * 16], k.d["hT"][:, :, tb:tb + M * 16], w=["hTc"])
                for s in range(16):
                    pp = ps[s % 2]; kp = "ps%d" % (s % 2)
                    for c in range(8):
                        mm(P, pp[0:M, 0:128], hTc[:, c, s:M * 16:16], Wu[:, c, fc * 128:(fc + 1) * 128], start=(c == 0), stop=(c == 7),
                           r=["hTc", "Wu"], w=[kp])
                    cp(P, "act", Uf[0:M, ct, s, :], pp[0:M, 0:128], r=[kp], w=["Uf"])
                    cp(P, "dve", Ub[0:M, ct, :, s, :], pp[0:M, 0:128].rearrange("p (g h) -> p g h", h=16), r=[kp], w=["Ub"])
                if k.debug and "u_d" in k.d:
                    P.dma(k.d["u_d"][tb:tb + M * 16, fc * 128:(fc + 1) * 128].rearrange("(c s) n -> c s n", s=16), Uf[0:M, ct], r=["Uf"])
                for g0 in range(0, 8, 4):
                    for g in range(g0, g0 + 4):
                        for sh in range(2):
                            slot = (g - g0) * 2 + sh
                            tr(P, psb[:, slot * 128:slot * 128 + M], Ub[0:M, ct, g, sh * 8:(sh + 1) * 8, :].rearrange("p s h -> p (s h)"),
                               k.identb[0:M, 0:M], r=["Ub", "identb"], w=["psb"])
                    cp(P, "act", Ug[:, g0:g0 + 4, :, col0:col0 + M],
                       psb[:].rearrange("p (g s c) -> p g s c", g=4, s=2)[:, :, :, 0:M], r=["psb"], w=["Ug"])
            P.emit(); esA.close()
            esB = contextlib.ExitStack(); sbB = _sb(nc, esB)
            dtb = sbB("dtb", [64, 2, 8]); lr = sbB("lr", [64, 16]); li = sbB("li", [64, 16]); bt = sbB("bt", [64, 2, 2, 8, 16])
            are = sbB("are", [64, 16, 80]); ys = sbB("ys", [64, 16, 80]); yc = sbB("yc", [64, 16, 80])
            yi = sbB("yi", [64, 16, 80], I32); yf = sbB("yf", [64, 16, 80])
            den = sbB("den", [64, 16]); am1 = sbB("am1", [64, 16]); fre = sbB("fre", [64, 16]); fim = sbB("fim", [64, 16]); ftmp = sbB("ftmp", [64, 16])
            btmp = sbB("btmp", [64, 16, 16])
            P.dma(lam[:], k.d["s5_lam"][:, :, :, fc * 8:(fc + 1) * 8], w=["lam"])
            P.dma(dtb[:], k.d["s5_logdt"].rearrange("o (d g) -> o d g", d=2)[:, :, fc * 8:(fc + 1) * 8].partition_broadcast(64), w=["dtb"])
            P.dma(bt[:], k.d["s5_b"][:, :, :, fc * 8:(fc + 1) * 8, :], w=["bt"])
            P.dma(ctl[:], k.d["s5_c"][:, :, :, fc * 8:(fc + 1) * 8, :], w=["ctl"])
            actf(P, dtb[:], dtb[:], AF.Exp, r=["dtb"], w=["dtb"])
            lr3 = lr[:].rearrange("p (d g) -> p d g", d=2); li3 = li[:].rearrange("p (d g) -> p d g", d=2)
            tt(P, "dve", lr3, lam[:, 0], dtb[:], ALU.mult, r=["lam", "dtb"], w=["lr"])
            tt(P, "dve", li3, lam[:, 1], dtb[:], ALU.mult, r=["lam", "dtb"], w=["li"])
            nr = nrow[:].rearrange("p a b -> p (a b)")
            tt(P, "dve", are[:], bc(lr[:].unsqueeze(2), [64, 16, 80]), bc(nr.unsqueeze(1), [64, 16, 80]), ALU.mult, r=["lr", "nrow"], w=["are"])
            tt(P, "dve", ys[:], bc(li[:].unsqueeze(2), [64, 16, 80]), bc(nr.unsqueeze(1), [64, 16, 80]), ALU.mult, r=["li", "nrow"], w=["ys"])
            actf(P, are[:], are[:], AF.Exp, r=["are"], w=["are"])
            ts(P, "dve", yc[:], ys[:], 1.0 / TWO_PI, 0.75, ALU.mult, ALU.add, r=["ys"], w=["yc"])
            ts(P, "dve", ys[:], ys[:], 1.0 / TWO_PI, 0.5, ALU.mult, ALU.add, r=["ys"], w=["ys"])
            sin_reduced(P, ys[:], ys[:], yi[:], yf[:], "ys", "yi", "yf", "ys")
            sin_reduced(P, yc[:], yc[:], yi[:], yf[:], "yc", "yi", "yf", "yc")
            Tre3 = Tre[:].rearrange("p q a b -> p q (a b)"); Tim3 = Tim[:].rearrange("p q a b -> p q (a b)")
            tt(P, "dve", Tre3, are[:], yc[:], ALU.mult, r=["are", "yc"], w=["Tre"])
            tt(P, "dve", Tim3, are[:], ys[:], ALU.mult, r=["are", "ys"], w=["Tim"])
            lre = lam[:, 0].rearrange("p d g -> p (d g)"); lim = lam[:, 1].rearrange("p d g -> p (d g)")
            a_re = Tre[:, :, 2, 0]; a_im = Tim[:, :, 2, 0]
            tt(P, "dve", den[:], lre, lre, ALU.mult, r=["lam"], w=["den"])
            tt(P, "dve", ftmp[:], lim, lim, ALU.mult, r=["lam"], w=["ftmp"])
            tt(P, "dve", den[:], den[:], ftmp[:], ALU.add, r=["den", "ftmp"], w=["den"])
            P.dve(lambda e: e.reciprocal(den[:], den[:]), r=["den"], w=["den"])
            ts(P, "dve", am1[:], a_re, -1.0, None, ALU.add, r=["Tre"], w=["am1"])
            tt(P, "dve", fre[:], am1[:], lre, ALU.mult, r=["am1", "lam"], w=["fre"])
            tt(P, "dve", ftmp[:], a_im, lim, ALU.mult, r=["Tim", "lam"], w=["ftmp"])
            tt(P, "dve", fre[:], fre[:], ftmp[:], ALU.add, r=["fre", "ftmp"], w=["fre"])
            tt(P, "dve", fre[:], fre[:], den[:], ALU.mult, r=["fre", "den"], w=["fre"])
            tt(P, "dve", fim[:], a_im, lre, ALU.mult, r=["Tim", "lam"], w=["fim"])
            tt(P, "dve", ftmp[:], am1[:], lim, ALU.mult, r=["am1", "lam"], w=["ftmp"])
            tt(P, "dve", fim[:], fim[:], ftmp[:], ALU.subtract, r=["fim", "ftmp"], w=["fim"])
            tt(P, "dve", fim[:], fim[:], den[:], ALU.mult, r=["fim", "den"], w=["fim"])
            b_re = bt[:, 0].rearrange("p d g h -> p (d g) h"); b_im = bt[:, 1].rearrange("p d g h -> p (d g) h")
            c_re = ctl[:, 0].rearrange("p d g h -> p (d g) h"); c_im = ctl[:, 1].rearrange("p d g h -> p (d g) h")
            fre_b = bc(fre[:].unsqueeze(2), [64, 16, 16]); fim_b = bc(fim[:].unsqueeze(2), [64, 16, 16])
            tt(P, "dve", Bre[:], b_re, fre_b, ALU.mult, r=["bt", "fre"], w=["Bre"])
            tt(P, "dve", btmp[:], b_im, fim_b, ALU.mult, r=["bt", "fim"], w=["btmp"])
            tt(P, "dve", Bre[:], Bre[:], btmp[:], ALU.subtract, r=["Bre", "btmp"], w=["Bre"])
            tt(P, "dve", Bim[:], b_im, fre_b, ALU.mult, r=["bt", "fre"], w=["Bim"])
            tt(P, "dve", btmp[:], b_re, fim_b, ALU.mult, r=["bt", "fim"], w=["btmp"])
            tt(P, "dve", Bim[:], Bim[:], btmp[:], ALU.add, r=["Bim", "btmp"], w=["Bim"])
            a16r = Tre[:, :, 2, 15].rearrange("p (d g) -> p d g", d=2); a16i = Tim[:, :, 2, 15].rearrange("p (d g) -> p d g", d=2)
            for r_ in range(2):
                cp(P, "dve", Ar2[:, :, r_, :], a16r, r=["Tre"], w=["Ar2"])
            ts(P, "dve", AiS[:, :, 0, :], a16i, -1.0, None, ALU.mult, r=["Tim"], w=["AiS"])
            cp(P, "dve", AiS[:, :, 1, :], a16i, r=["Tim"], w=["AiS"])
            P.emit(); esB.close()
            esC = contextlib.ExitStack(); sbC = _sb(nc, esC)
            Z = sbC("Z", [64, 2 * 273 * 16]); Zbf = sbC("Zbf", [64, 2 * 273 * 16], BF16)
            Tsb = sbC("Tsb", [128, 16, 2, 256], BF16); Msb = sbC("Msb", [64, 16, 2, 256], BF16)
            WT = sbC("WT", [128, 16, 2, 2, 64], BF16); Ysb = sbC("Ysb", [128, 2, 272], BF16)
            g1 = [sbC("g1_%d" % i, [64, 16, 16]) for i in range(4)]
            Xre = sbC("Xre", [64, 256]); Xim = sbC("Xim", [64, 256]); Yre = sbC("Yre", [64, 256]); YimN = sbC("YimN", [64, 256])
            Wre = sbC("Wre", [64, 256]); Wim = sbC("Wim", [64, 256])
            t1 = sbC("t1", [64, 2, 16]); t2 = sbC("t2", [64, 2, 16])
            zb = Z[:]
            pst = zb.ap[0][0]

            def zap(a, b_, sub=None, zb=zb, pst=pst):
                off = zb.offset + a * 16
                dstride = (273 + b_ - a) * 16
                if sub is None:
                    return bass.AP(zb.tensor, off, [[pst, 64], [dstride, 2], [1, 16]])
                return bass.AP(zb.tensor, off + 8 * sub, [[pst, 64], [dstride, 2], [1, 8]])

            Z5 = Z[:].rearrange("p (d q r g) -> p d q r g", d=2, q=273, r=2)
            Zb5 = Zbf[:].rearrange("p (d q r g) -> p d q r g", d=2, q=273, r=2)
            P.pool(lambda e: e.memset(Z[:], 0.0), w=["Z"])

            def cprod(eng, ore, oim, are_, aim_, bre_, bim_, negim, r, w):
                tt(P, eng, g1[0][:], are_, bre_, ALU.mult, r=r, w=["g1_0"])
                tt(P, eng, g1[1][:], aim_, bim_, ALU.mult, r=r, w=["g1_1"])
                tt(P, eng, ore, g1[0][:], g1[1][:], ALU.subtract, r=["g1_0", "g1_1"], w=w)
                tt(P, eng, g1[2][:], are_, bim_, ALU.mult, r=r, w=["g1_2"])
                tt(P, eng, g1[3][:], aim_, bre_, ALU.mult, r=r, w=["g1_3"])
                if negim:
                    stt(P, eng, oim, g1[2][:], -1.0, g1[3][:], ALU.mult, ALU.subtract, r=["g1_2", "g1_3"], w=w)
                else:
                    tt(P, eng, oim, g1[2][:], g1[3][:], ALU.add, r=["g1_2", "g1_3"], w=w)

            v3 = lambda t_: t_[:].rearrange("p (a b) -> p a b", b=16)
            for g in range(8):
                for di in range(2):
                    q = di * 8 + g
                    tbx, tby, tbm, tbw = (1, 0, 2, 3) if di == 0 else (0, 1, 4, 0)
                    rd = ["Bre", "Bim", "Tre", "Tim", "ctl"]
                    Bre_b = bc(Bre[:, q, :].unsqueeze(1), [64, 16, 16]); Bim_b = bc(Bim[:, q, :].unsqueeze(1), [64, 16, 16])
                    Cre_b = bc(c_re[:, q, :].unsqueeze(1), [64, 16, 16]); Cim_b = bc(c_im[:, q, :].unsqueeze(1), [64, 16, 16])
                    pw = lambda T_, tb_: bc(T_[:, q, tb_, :].unsqueeze(2), [64, 16, 16])
                    cprod("dve", v3(Xre), v3(Xim), Bre_b, Bim_b, pw(Tre, tbx), pw(Tim, tbx), False, rd, ["Xre"])
                    cprod("dve", v3(Yre), v3(YimN), Cre_b, Cim_b, pw(Tre, tby), pw(Tim, tby), True, rd, ["Yre"])
                    for sh in range(2):
                        pp = ps[2 + sh]; kp = "ps%d" % (2 + sh)
                        mm(P, pp[:, 0:256], Xre[:, sh * 128:(sh + 1) * 128], Yre[:], start=True, stop=False, r=["Xre", "Yre"], w=[kp])
                        mm(P, pp[:, 0:256], Xim[:, sh * 128:(sh + 1) * 128], YimN[:], start=False, stop=True, r=["Xre", "Yre"], w=[kp])
                        tt(P, "dve", Tsb[:, q, sh, :], pp[:, 0:256], mT[:, di, sh, :], ALU.mult, r=[kp, "mT"], w=["Tsb"])
                    cprod("dve", v3(Yre), v3(YimN), Cre_b, Cim_b, pw(Tre, tbm), pw(Tim, tbm), True, rd, ["Yre"])
                    cp(P, "act", Msb[:, q, 0, :], Yre[:], r=["Yre"], w=["Msb"])
                    cp(P, "act", Msb[:, q, 1, :], YimN[:], r=["Yre"], w=["Msb"])
                    cprod("dve", v3(Wre), v3(Wim), Bre_b, Bim_b, pw(Tre, tbw), pw(Tim, tbw), False, rd, ["Wre"])
                    for ri, Wt in enumerate((Wre, Wim)):
                        for sh in range(2):
                            tr(P, ps[4][:, (ri * 2 + sh) * 64:(ri * 2 + sh + 1) * 64], Wt[:, sh * 128:(sh + 1) * 128], k.identf[0:64, 0:64],
                               r=["Wre", "identf"], w=["ps4"])
                    cp(P, "act", WT[:, q].rearrange("p r s c -> p (r s c)"), ps[4][:, 0:256], r=["ps4"], w=["WT"])
                    for ri in range(2):
                        pp = ps[5 + ri]; kp = "ps%d" % (5 + ri)
                        for sh in range(2):
                            mm(P, pp[0:64, 0:272], WT[:, q, ri, sh, :], Ug[:, g, sh, :], start=(sh == 0), stop=(sh == 1), r=["WT", "Ug"], w=[kp])
                        if di == 0:
                            cp(P, "act", Z5[:, 0, 17:273, ri, g], pp[0:64, 0:256], r=[kp], w=["Z"])
                            cp(P, "act", Z5[:, 0, 1:17, ri, g], pp[0:64, 256:272], r=[kp], w=["Z"])
                        else:
                            cp(P, "act", Z5[:, 1, 0:272, ri, g], pp[0:64, 0:272], r=[kp], w=["Z"])
            Ar2v = Ar2[:].rearrange("p d r g -> p d (r g)")
            for kk in range(1, 272):
                prev = zap(kk, 272 - kk); cur = zap(1 + kk, 271 - kk)
                tt(P, "dve", t1[:], prev, Ar2v, ALU.mult, r=["Z", "Ar2"], w=["t1"])
                tt(P, "dve", t2[:, :, 0:8], zap(kk, 272 - kk, 1), AiS[:, :, 0, :], ALU.mult, r=["Z", "AiS"], w=["t2"])
                tt(P, "dve", t2[:, :, 8:16], zap(kk, 272 - kk, 0), AiS[:, :, 1, :], ALU.mult, r=["Z", "AiS"], w=["t2"])
                tt(P, "dve", cur, cur, t1[:], ALU.add, r=["Z", "t1"], w=["Z"])
                tt(P, "dve", cur, cur, t2[:], ALU.add, r=["Z", "t2"], w=["Z"])
            cp(P, "act", Zbf[:], Z[:], r=["Z"], w=["Zbf"])
            for g in range(8):
                for th in range(2):
                    pp = ps[th]; kp = "ps%d" % th
                    first = True
                    for di in range(2):
                        q = di * 8 + g
                        for sh in range(2):
                            if (di == 0 and sh <= th) or (di == 1 and sh >= th):
                                mm(P, pp[:, 0:272], Tsb[:, q, sh, th * 128:(th + 1) * 128], Ug[:, g, sh, :], start=first, stop=False,
                                   r=["Tsb", "Ug"], w=[kp])
                                first = False
                    for di in range(2):
                        q = di * 8 + g
                        for ri in range(2):
                            lhs = Msb[:, q, ri, th * 128:(th + 1) * 128]
                            last = (di == 1 and ri == 1)
                            if di == 0:
                                mm(P, pp[:, 0:256], lhs, Zb5[:, 0, 16:272, ri, g], start=False, stop=last, r=["Msb", "Zbf"], w=[kp])
                                mm(P, pp[:, 256:272], lhs, Zb5[:, 0, 0:16, ri, g], start=False, stop=last, r=["Msb", "Zbf"], w=[kp])
                            else:
                                mm(P, pp[:, 0:256], lhs, Zb5[:, 1, 1:257, ri, g], start=False, stop=last, r=["Msb", "Zbf"], w=[kp])
                                mm(P, pp[:, 256:272], lhs, Zb5[:, 1, 257:273, ri, g], start=False, stop=last, r=["Msb", "Zbf"], w=[kp])
                    cp(P, "act", Ysb[:, th, :], pp[:, 0:272], r=[kp], w=["Ysb"])
                for ct, (tb, M, col0) in enumerate(CT):
                    for th in range(2):
                        tr(P, psb[0:M, th * 128:(th + 1) * 128], Ysb[:, th, col0:col0 + M], k.identb[:], r=["Ysb", "identb"], w=["psb"])
                    cp(P, "dve", Yp[0:M, ct, :, g * 16:(g + 1) * 16], psb[0:M, 0:256].rearrange("p (t h) -> p t h", h=16), r=["psb"], w=["Yp"])
            P.emit(); esC.close()
            esD = contextlib.ExitStack(); sbD = _sb(nc, esD)
            s32 = sbD("s32", [128, 16, 128]); gx = sbD("gx", [128, 16, 128]); gq = sbD("gq", [128, 16, 128])
            abf = sbD("abf", [128, 16, 128], BF16); aTt = sbD("aTt", [128, 2048], BF16)
            for ct, (tb, M, col0) in enumerate(CT):
                dsl = bc(dbc[0:M, fc * 128:(fc + 1) * 128].unsqueeze(1), [M, 16, 128])
                tt(P, "pool", s32[0:M], Uf[0:M, ct], dsl, ALU.mult, r=["Uf", "dbc"], w=["s32"])
                tt(P, "dve", s32[0:M], s32[0:M], Yp[0:M, ct], ALU.add, r=["s32", "Yp"], w=["s32"])
                if k.debug and "dbg_s" in k.d:
                    P.dma(k.d["dbg_s"][tb:tb + M * 16, fc * 128:(fc + 1) * 128].rearrange("(c s) n -> c s n", s=16), s32[0:M], r=["s32"])
                tt(P, "pool", gx[0:M], s32[0:M], s32[0:M], ALU.mult, r=["s32"], w=["gx"])
                ts(P, "dve", gx[0:M], gx[0:M], 0.044715, 1.0, ALU.mult, ALU.add, r=["gx"], w=["gx"])
                tt(P, "pool", gq[0:M], gx[0:M], s32[0:M], ALU.mult, r=["gx", "s32"], w=["gq"])
                actf(P, gq[0:M], gq[0:M], AF.Sigmoid, r=["gq"], w=["gq"], scale=1.5957691216057308)
                tt(P, "dve", abf[0:M], s32[0:M], gq[0:M], ALU.mult, r=["s32", "gq"], w=["abf"])
                aT3 = aTt[:, 0:M * 16].rearrange("p (c t) -> p t c", t=16)
                for t0 in range(0, 16, 8):
                    for t_ in range(t0, t0 + 8):
                        tr(P, psb[:, (t_ - t0) * 128:(t_ - t0) * 128 + M], abf[0:M, t_, :], k.identb[0:M, 0:M], r=["abf", "identb"], w=["psb"])
                    cp(P, "act", aT3[:, t0:t0 + 8, :], psb[:].rearrange("p (t c) -> p t c", c=128)[:, :, 0:M], r=["psb"], w=["aTt"])
                P.dma(k.d["s5oT"][:, fc, tb:tb + M * 16], aTt[:, 0:M * 16], r=["aTt"], w=["s5oT_a"])
            P.emit(); esD.close()
    with contextlib.ExitStack() as es:
        sb = _sb(nc, es)
        gw = sb("gw", [128, 4, 512], BF16); gb = sb("gb", [128, 4])
        P.dma(gw[:], k.d["glu_w"].rearrange("(c p) n -> p c n", p=128), w=["gw"], q="pool")
        P.dma(gb[:], k.d["glu_b"], w=["gb"])
        aT = [sb("aT%d" % i, [128, 4, 512], BF16) for i in range(2)]
        sg_ = [sb("sgz%d" % i, [128, 4, 512]) for i in range(2)]
        oT = [sb("oT%d" % i, [128, 4, 512], BF16) for i in range(2)]
        for bi, t0 in enumerate(range(0, T, 512)):
            n = min(512, T - t0)
            b = bi % 2
            a_, ka = aT[b], "aT%d" % b
            P.dma(a_[:, :, 0:n], k.d["s5oT"][:, :, t0:t0 + n], w=[ka])
            for oc in range(4):
                pp = ps[oc]; kp = "ps%d" % oc
                for kc in range(4):
                    mm(P, pp[:, 0:n], gw[:, kc, oc * 128:(oc + 1) * 128], a_[:, kc, 0:n], start=(kc == 0), stop=(kc == 3), r=["gw", ka], w=[kp])
                actf(P, sg_[b][:, oc, 0:n], pp[:, 0:n], AF.Sigmoid, r=[kp], w=["sgz%d" % b], sr=["gb"], bias=gb[:, oc:oc + 1])
            tt(P, "dve", oT[b][:, :, 0:n], a_[:, :, 0:n], sg_[b][:, :, 0:n], ALU.mult, r=[ka, "sgz%d" % b], w=["oT%d" % b])
            P.dma(k.d["s5oT"][:, :, t0:t0 + n], oT[b][:, :, 0:n], r=["oT%d" % b], w=["s5oT_%d" % bi])
        P.emit()
```

```python
import contextlib
import math
import numpy as np
import concourse.bass as bass
import concourse.mybir as mybir
from concourse.bass_utils import run_bass_kernel_spmd

F32 = mybir.dt.float32
BF16 = mybir.dt.bfloat16
I32 = mybir.dt.int32
ALU = mybir.AluOpType
AF = mybir.ActivationFunctionType
AX = mybir.AxisListType

COMPUTE = ("pe", "act", "dve", "pool")
RELAX = {"dve": 256, "pool": 256, "act": 512}
N_DMA_SEMS = 12
D = 1024
NCTX = 256
NLAT = 4096
T = NCTX + NLAT
NT = T // 128
EPS = 1e-6


class Prog:
    def __init__(self, nc, es):
        self.nc = nc
        self.csem = {e: es.enter_context(nc.semaphore("s_" + e)) for e in COMPUTE}
        self.dsem = {q: [es.enter_context(nc.semaphore("d_%s%d" % (q, i))) for i in range(N_DMA_SEMS)]
                     for q in ("sp", "pool")}
        self.cnt = {e: 0 for e in COMPUTE}
        self.ndma = {"sp": 0, "pool": 0}
        self.begin()

    def begin(self):
        self.ops = []
        self.state = {}
        self.lastdma = {}

    def capture(self, f):
        self.cap = []
        f()
        out, self.cap = self.cap, None
        return out

    def replay(self, lst):
        for a in lst:
            self._add(*a)

    def merge(self, A, B):
        na, nb = len(A), len(B)
        ia = ib = 0
        while ia < na or ib < nb:
            if ib >= nb or (ia < na and ia * nb <= ib * na):
                self._add(*A[ia]); ia += 1
            else:
                self._add(*B[ib]); ib += 1

    def _add(self, eng, kind, fn, reads, writes, sreads=(), n=0):
        if getattr(self, "cap", None) is not None:
            self.cap.append((eng, kind, fn, tuple(reads), tuple(writes), tuple(sreads), n))
            return None
        op = dict(eng=eng, kind=kind, fn=fn, deps=[], idx=len(self.ops), signal=False, n=n)
        deps = set()
        strict = set()
        reads = tuple(reads) + tuple(sreads)
        for k in reads:
            st = self.state.setdefault(k, [None, []])
            if st[0] is not None:
                deps.add(st[0]["idx"])
                if eng != "pe" and (k in sreads or st[0].get("n", 0) < RELAX.get(eng, 1 << 30)):
                    strict.add(st[0]["idx"])
        for k in tuple(writes) + tuple(kk for kk in reads if isinstance(kk, str) and kk.startswith("ps")):
            st = self.state.setdefault(k, [None, []])
            if st[0] is not None:
                deps.add(st[0]["idx"])
            for r in st[1]:
                deps.add(r["idx"])
        if kind == "dma":
            n = self.ndma[eng]
            self.ndma[eng] += 1
            op["dslot"] = n % N_DMA_SEMS
            op["dround"] = n // N_DMA_SEMS + 1
            prev = self.lastdma.get((eng, op["dslot"]))
            if prev is not None:
                deps.add(prev["idx"])
            self.lastdma[(eng, op["dslot"])] = op
        for d in sorted(deps):
            o = self.ops[d]
            if o["kind"] == "compute" and o["eng"] == eng and d not in strict:
                continue
            op["deps"].append(d)
            o["signal"] = True
        for k in reads:
            self.state[k][1].append(op)
        for k in writes:
            self.state[k] = [op, []]
        self.ops.append(op)
        return op

    def pe(self, fn, r=(), w=(), sr=(), n=0):
        return self._add("pe", "compute", fn, r, w, sr, n)

    def act(self, fn, r=(), w=(), sr=(), n=0):
        return self._add("act", "compute", fn, r, w, sr, n)

    def dve(self, fn, r=(), w=(), sr=(), n=0):
        return self._add("dve", "compute", fn, r, w, sr, n)

    def pool(self, fn, r=(), w=(), sr=(), n=0):
        return self._add("pool", "compute", fn, r, w, sr, n)

    def eng(self, name):
        return {"pe": self.pe, "act": self.act, "dve": self.dve, "pool": self.pool}[name]

    def dma(self, out, in_, r=(), w=(), q="sp", **kw):
        return self._add(q, "dma", lambda e: e.dma_start(out=out, in_=in_, **kw), r, w)

    def emit(self):
        nc = self.nc
        csem, dsem, cnt = self.csem, self.dsem, self.cnt
        for op in self.ops:
            if op["kind"] == "compute" and op["signal"]:
                cnt[op["eng"]] += 1
                op["count"] = cnt[op["eng"]]
        last_dma = dict(self.lastdma)
        with nc.Block() as block:
            engs = {"pe": block.tensor, "act": block.scalar, "dve": block.vector,
                    "pool": block.gpsimd, "sp": block.sync}
            for ename, deco in engs.items():
                myops = [o for o in self.ops if o["eng"] == ename]

                def body(e, myops=myops, ename=ename):
                    waited = {}
                    for op in myops:
                        for d in op["deps"]:
                            o = self.ops[d]
                            if o["kind"] == "compute":
                                key = ("c", o["eng"]); val = o["count"]; sem = csem[o["eng"]]
                            else:
                                key = ("d", o["eng"], o["dslot"]); val = 16 * o["dround"]
                                sem = dsem[o["eng"]][o["dslot"]]
                            if waited.get(key, 0) >= val:
                                continue
                            waited[key] = val
                            e.wait_ge(sem, val)
                        ins = op["fn"](e)
                        if op["kind"] == "dma":
                            ins.then_inc(dsem[op["eng"]][op["dslot"]], 16)
                        elif op["signal"]:
                            ins.then_inc(csem[op["eng"]], 1)
                    if ename == "sp":
                        for (q, s_), o in last_dma.items():
                            e.wait_ge(dsem[q][s_], 16 * o["dround"])
                deco(body)
        nc.all_engine_barrier()
        self.begin()


class K:
    pass


_UID = [0]


def _sb(nc, es):
    def f(name, shape, dt=F32):
        _UID[0] += 1
        return es.enter_context(nc.sbuf_tensor("sb%d_%s" % (_UID[0], name), list(shape), dt))
    return f


def mm(P, out, lhsT, rhs, start=True, stop=True, r=(), w=()):
    P.pe(lambda e: e.matmul(out, lhsT, rhs, start=start, stop=stop), r, w)


def tr(P, out, in_, ident, r=(), w=()):
    P.pe(lambda e: e.transpose(out, in_, ident), r, w)


def _n(ap):
    try:
        return int(ap.free_size())
    except Exception:
        return 0


def actf(P, out, in_, func, r=(), w=(), sr=(), **kw):
    P.act(lambda e: e.activation(out, in_, func, **kw), r, w, sr, n=_n(out))


def tt(P, eng, out, in0, in1, op, r=(), w=()):
    P.eng(eng)(lambda e: e.tensor_tensor(out, in0, in1, op), r, w, n=_n(out))


def ts(P, eng, out, in0, s1, s2, op0, op1=None, r=(), w=(), sr=()):
    if op1 is None:
        P.eng(eng)(lambda e: e.tensor_scalar(out, in0, s1, None, op0), r, w, sr, n=_n(out))
    else:
        P.eng(eng)(lambda e: e.tensor_scalar(out, in0, s1, s2, op0, op1), r, w, sr, n=_n(out))


def stt(P, eng, out, in0, scalar, in1, op0, op1, r=(), w=(), sr=()):
    P.eng(eng)(lambda e: e.scalar_tensor_tensor(out, in0, scalar, in1, op0, op1), r, w, sr, n=_n(out))


def cp(P, eng, out, in_, r=(), w=()):
    if eng == "act":
        P.act(lambda e: e.copy(out, in_), r, w, n=_n(out))
    else:
        P.eng(eng)(lambda e: e.tensor_copy(out, in_), r, w, n=_n(out))


def red(P, eng, out, in_, op, r=(), w=()):
    P.eng(eng)(lambda e: e.tensor_reduce(out, in_, AX.X, op), r, w)


def bc(ap, shape):
    return ap.to_broadcast(list(shape))


def alloc_consts(k):
    sb = k.sbp
    k.identf = sb("identf", [128, 128])
    k.identb = sb("identb", [128, 128], BF16)
    k.maskf = sb("maskf", [128, 128])
    k.maskb = sb("maskb", [128, 128])
    k.scanm = sb("scanm", [128, 128])
    k.sel = sb("sel", [32, 32, 128], BF16)
    k.cols = sb("cols", [128, 2, 4, 8, 2])
    k.posB = sb("posB", [128, 512])


def stage_consts(k, sbt):
    nc, P = k.nc, k.P
    P.pool(lambda e: e.memset(k.identf[:], 1.0), w=["identf"])
    P.pool(lambda e: e.affine_select(k.identf[:], k.identf[:], [[-1, 128]], ALU.is_equal, 0.0, base=0,
                                     channel_multiplier=1), r=["identf"], w=["identf"])
    cp(P, "dve", k.identb[:], k.identf[:], r=["identf"], w=["identb"])
    for name, tile_, sgn in (("maskf", k.maskf, 1), ("maskb", k.maskb, -1)):
        P.pool(lambda e, t=tile_: e.memset(t[:], 1.0), w=[name])
        P.pool(lambda e, t=tile_, c=sgn: e.affine_select(t[:], t[:], [[c, 128]], ALU.is_ge, 0.0, base=0,
                                                        channel_multiplier=-c), r=[name], w=[name])
        P.pool(lambda e, t=tile_: e.memset(t[0:64, 64:128], 0.0), r=[name], w=[name])
        P.pool(lambda e, t=tile_: e.memset(t[64:128, 0:64], 0.0), r=[name], w=[name])
    P.pool(lambda e: e.memset(k.scanm[:], 1.0), w=["scanm"])
    P.pool(lambda e: e.memset(k.scanm[:, 0:1], 0.0), r=["scanm"], w=["scanm"])
    P.pool(lambda e: e.memset(k.scanm[:, 64:65], 0.0), r=["scanm"], w=["scanm"])
    k.self32 = sbt("self32", [32, 32, 128])
    P.pool(lambda e: e.memset(k.self32[:], 1.0), w=["self32"])
    P.pool(lambda e: e.affine_select(k.self32[:], k.self32[:], [[-1, 32], [0, 128]], ALU.is_equal, 0.0, base=0,
                                     channel_multiplier=1), r=["self32"], w=["self32"])
    cp(P, "dve", k.sel[:], k.self32[:], r=["self32"], w=["sel"])


def stage_prologue(k):
    nc, P = k.nc, k.P
    with contextlib.ExitStack() as es:
        sb = _sb(nc, es)
        stage_consts(k, sb)
        csv = sb("csv", [128, 8, 2]); sv = sb("sv", [128, 8, 2])
        nmix = sb("nmix", [128, 2, 8]); nffn = sb("nffn", [128, 2, 8])
        P.dma(csv[:], k.d["csv"], w=["csv"])
        P.dma(nmix[:], k.d["nmix"], w=["nmix"])
        P.dma(nffn[:], k.d["nffn"], w=["nffn"])
        actf(P, sv[:], csv[:], AF.Silu, r=["csv"], w=["sv"])
        aw = [sb("aw%d" % i, [128, 3072]) for i in range(2)]
        modrow = sb("modrow", [2, 6144]); adab = sb("adab", [2, 6144])
        modcol = sb("modcol", [128, 48, 2])
        ps = k.ps
        for l in range(2):
            P.dma(adab[:], k.d["ada_b"][l:l + 1, :].partition_broadcast(2), w=["adab"])
            it = 0
            for ch in range(2):
                for kc in range(8):
                    a = aw[it % 2]; akey = "aw%d" % (it % 2); it += 1
                    P.dma(a[:], k.d["ada_w"][l, kc * 128:(kc + 1) * 128, ch * 3072:(ch + 1) * 3072], w=[akey])
                    for j in range(6):
                        mm(P, ps[j][0:2, :], sv[:, kc, :], a[:, j * 512:(j + 1) * 512], start=(kc == 0), stop=(kc == 7),
                           r=["sv", akey], w=["ps%d" % j])
                for j in range(6):
                    c0 = (ch * 6 + j) * 512
                    tt(P, "dve", modrow[:, c0:c0 + 512], ps[j][0:2, :], adab[:, c0:c0 + 512], ALU.add,
                       r=["ps%d" % j, "adab"], w=["modrow"])
            P.dma(k.d["modd"][l], modrow[:], r=["modrow"], w=["modd"])
            pst = ps[6][:, 0:96].rearrange("p (c j) -> p c j", j=2)
            for c in range(48):
                tr(P, pst[:, c, :], modrow[0:2, c * 128:(c + 1) * 128], k.identf[0:2, 0:2], r=["modrow", "identf"], w=["ps6"])
            cp(P, "dve", modcol[:], pst, r=["ps6"], w=["modcol"])
            C = k.cols
            for (ai, bi, nrm, sc0, sh0) in ((0, 1, nmix, 8, 0), (2, 3, nffn, 32, 24)):
                ts(P, "dve", C[:, l, ai], modcol[:, sc0:sc0 + 8, :], 1.0, None, ALU.add, r=["modcol"], w=["cols"])
                tt(P, "dve", C[:, l, ai], C[:, l, ai], bc(nrm[:, l, :].unsqueeze(2), [128, 8, 2]), ALU.mult,
                   r=["cols", "nmix", "nffn"], w=["cols"])
                cp(P, "dve", C[:, l, bi], modcol[:, sh0:sh0 + 8, :], r=["modcol"], w=["cols"])
        io = sb("io", [128, 256], I32); om = sb("om", [128, 256]); pc_i = sb("pc_i", [128, 1], I32)
        pc = sb("pc", [128, 1]); pm = sb("pm", [128, 1]); ang = sb("ang", [128, 256])
        y = sb("yy", [128, 512]); yi = sb("yi", [128, 512], I32); yf = sb("yf", [128, 512])
        P.pool(lambda e: e.iota(io[:], [[1, 256]], base=0, channel_multiplier=0), w=["io"])
        P.pool(lambda e: e.iota(pc_i[:], [[0, 1]], base=0, channel_multiplier=1), w=["pc_i"])
        cp(P, "dve", om[:], io[:], r=["io"], w=["om"])
        P.dve(lambda e: e.tensor_single_scalar(pc_i[:], pc_i[:], 63, ALU.bitwise_and), r=["pc_i"], w=["pc_i"])
        cp(P, "pool", pc[:], pc_i[:], r=["pc_i"], w=["pc"])
        actf(P, om[:], om[:], AF.Exp, r=["om"], w=["om"], scale=float(-math.log(10000.0) / 256.0))
        ts(P, "dve", ang[:], om[:], pc[:, 0:1], None, ALU.mult, r=["om"], sr=["pc"], w=["ang"])
        ts(P, "dve", y[:, 0:256], ang[:], float(1 / (2 * math.pi)), 0.5, ALU.mult, ALU.add, r=["ang"], w=["yy"])
        ts(P, "dve", y[:, 256:512], ang[:], float(1 / (2 * math.pi)), 0.75, ALU.mult, ALU.add, r=["ang"], w=["yy"])
        sin_reduced(P, k.posB[:], y[:], yi[:], yf[:], "yy", "yi", "yf", "posB")
        P.dma(k.d["t64d"], k.posB[0:64, :], r=["posB"], w=["t64d"])
        P.emit()


def sin_reduced(P, out, y, yi, yf, ky, kyi, kyf, kout):
    cp(P, "dve", yi, y, r=[ky], w=[kyi])
    cp(P, "dve", yf, yi, r=[kyi], w=[kyf])
    tt(P, "dve", y, y, yf, ALU.subtract, r=[ky, kyf], w=[ky])
    P.dve(lambda e: e.tensor_single_scalar(yf, y, 0.0, ALU.is_lt), r=[ky], w=[kyf])
    tt(P, "dve", y, y, yf, ALU.add, r=[ky, kyf], w=[ky])
    actf(P, out, y, AF.Sin, r=[ky], w=[kout], bias=float(-math.pi), scale=float(2 * math.pi))


def norm_tile(k, xt, kx, A, Bc, hout, khout, sfx=""):
    P = k.P
    junk, ss = k.njunk, k.nss
    actf(P, junk[:], xt, AF.Square, r=[kx], w=["njunk"])
    red(P, "dve", ss[:], junk[:], ALU.add, r=["njunk"], w=["nss"])
    ts(P, "dve", ss[:], ss[:], 1.0 / D, EPS, ALU.mult, ALU.add, r=["nss"], w=["nss"])
    actf(P, ss[:], ss[:], AF.Sqrt, r=["nss"], w=["nss"])
    P.dve(lambda e: e.reciprocal(ss[:], ss[:]), r=["nss"], w=["nss"])
    ts(P, "dve", junk[:], xt, ss[:, 0:1], None, ALU.mult, r=[kx], sr=["nss"], w=["njunk"])
    pa = k.ps[0][:].rearrange("p (c t) -> p c t", t=128)
    pb = k.ps[1][:].rearrange("p (c t) -> p c t", t=128)
    for c in range(8):
        dst = pa[:, c, :] if c < 4 else pb[:, c - 4, :]
        tr(P, dst, junk[:, c * 128:(c + 1) * 128], k.identf[:], r=["njunk", "identf"], w=["ps%d" % (c // 4)])
    for half, pp in ((0, pa), (1, pb)):
        tt(P, "dve", hout[:, half * 4:half * 4 + 4, :], pp, bc(A[:, half * 4:half * 4 + 4].unsqueeze(2), [128, 4, 128]),
           ALU.mult, r=["ps%d" % half, "cols"], w=[khout])
        tt(P, "dve", hout[:, half * 4:half * 4 + 4, :], hout[:, half * 4:half * 4 + 4, :],
           bc(Bc[:, half * 4:half * 4 + 4].unsqueeze(2), [128, 4, 128]), ALU.add, r=[khout, "cols"], w=[khout])


def stage_A(k, l, es_ext=None):
    nc, P = k.nc, k.P
    with contextlib.ExitStack() as es_own:
        es = es_ext if es_ext is not None else es_own
        sb = _sb(nc, es)
        k.njunk = sb("njunk", [128, 1024]); k.nss = sb("nss", [128, 1])
        xt = [sb("xt%d" % i, [128, 1024]) for i in range(2)]
        pa = [sb("pa%d" % i, [128, 512]) for i in range(2)]
        h32 = [sb("h32_%d" % i, [128, 8, 128]) for i in range(2)]
        hb = [sb("hb%d" % i, [128, 8, 128], BF16) for i in range(2)]
        for i in range(NT):
            b = i % 2
            x, kx = xt[b], "xt%d" % b
            lat = i >= 2
            if l == 0:
                P.dma(x[:], k.d["xin"][i * 128:(i + 1) * 128, :], w=[kx])
                if lat:
                    r0 = (i - 2) * 2
                    P.dma(pa[b][0:64, :], k.d["t64d"][r0:r0 + 1, :].partition_broadcast(64), r=["t64d"], w=["pa%d" % b])
                    P.dma(pa[b][64:128, :], k.d["t64d"][r0 + 1:r0 + 2, :].partition_broadcast(64), r=["t64d"], w=["pa%d" % b])
                    tt(P, "pool", x[:, 0:512], x[:, 0:512], pa[b][:], ALU.add, r=[kx, "pa%d" % b], w=[kx])
                    tt(P, "pool", x[:, 512:1024], x[:, 512:1024], k.posB[:], ALU.add, r=[kx, "posB"], w=[kx])
                P.dma(k.d["X"][i * 128:(i + 1) * 128, :], x[:], r=[kx], w=["X%d" % i])
            else:
                P.dma(x[:], k.d["X"][i * 128:(i + 1) * 128, :], r=["X%d" % i], w=[kx])
            j = 0 if lat else 1
            norm_tile(k, x[:], kx, k.cols[:, l, 0, :, j], k.cols[:, l, 1, :, j], h32[b], "h32_%d" % b)
            cp(P, "act", hb[b][:], h32[b][:], r=["h32_%d" % b], w=["hb%d" % b])
            P.dma(k.d["hT"][:, :, i * 128:(i + 1) * 128], hb[b][:], r=["hb%d" % b], w=["hT%d" % i])
        if es_ext is None:
            P.emit()


def stage_precast(k, l, es_ext=None):
    nc, P = k.nc, k.P
    with contextlib.ExitStack() as es_own:
        es = es_ext if es_ext is not None else es_own
        sb = _sb(nc, es)
        st = [sb("st%d" % i, [128, 8, 512]) for i in range(3)]
        sd = [sb("sd%d" % i, [128, 2, 1024]) for i in range(3)]
        bt_ = [sb("bt%d" % i, [128, 8, 512], BF16) for i in range(3)]
        bd = [sb("bd%d" % i, [128, 2, 1024], BF16) for i in range(3)]
        engs = ("act", "dve", "pool")
        for e_ in range(32):
            b = e_ % 3
            P.dma(st[b][:, :, 0:256], k.d["moe_wg"][l, e_].rearrange("(c p) n -> p c n", p=128), w=["st%d" % b])
            P.dma(st[b][:, :, 256:512], k.d["moe_wu"][l, e_].rearrange("(c p) n -> p c n", p=128), w=["st%d" % b])
            P.dma(sd[b][:], k.d["moe_wd"][l, e_].rearrange("(c p) n -> p c n", p=128), w=["sd%d" % b])
            cp(P, engs[e_ % 3], bt_[b][:], st[b][:], r=["st%d" % b], w=["bt%d" % b])
            cp(P, engs[(e_ + 1) % 3], bd[b][:], sd[b][:], r=["sd%d" % b], w=["bd%d" % b])
            P.dma(k.d["wgu_bf"][l, e_], bt_[b][:], r=["bt%d" % b], w=["wgu_o%d" % e_])
            P.dma(k.d["wd_bf"][l, e_], bd[b][:], r=["bd%d" % b], w=["wd_o%d" % e_])
        if es_ext is None:
            P.emit()


def stage_AC(k, l):
    P = k.P
    with contextlib.ExitStack() as es:
        a = P.capture(lambda: stage_A(k, l, es))
        c = P.capture(lambda: stage_precast(k, l, es))
        P.merge(a, c)
        P.emit()


def stage_moe(k, l, final):
    nc, P = k.nc, k.P
    ps = k.ps
    tiles = list(range(NT)) if l == 0 else list(range(2, NT))
    blocks = []
    if l == 0:
        blocks.append([0, 1])
    for b0 in range(2, NT, 8):
        blocks.append(list(range(b0, b0 + 8)))
    with contextlib.ExitStack() as es:
        sb = _sb(nc, es)
        k.njunk = sb("njunk", [128, 1024]); k.nss = sb("nss", [128, 1])
        xt = [sb("xt%d" % i, [128, 1024]) for i in range(2)]
        h32 = [sb("h32_%d" % i, [128, 8, 128]) for i in range(2)]
        h2T = sb("h2T", [128, 8, 1024], BF16)
        acc = sb("acc", [128, 8, 1024])
        gT = sb("gT", [32, 1024], BF16)
        rw = sb("rw", [128, 8, 32]); rb = sb("rb", [128, 32])
        g2bc = sb("g2bc", [128, 2, 1024]); fnbc = sb("fnbc", [128, 1024])
        wgu = [sb("wgu%d" % i, [128, 8, 512], BF16) for i in range(3)]
        wd = [sb("wd%d" % i, [128, 2, 1024], BF16) for i in range(8)]
        G = [sb("G%d" % i, [128, 2, 1024], BF16) for i in range(8)]
        sg = [sb("sg%d" % i, [128, 2, 256]) for i in range(2)]
        gbs = [sb("gbs%d" % i, [128, 256]) for i in range(2)]
        lg = sb("lg", [128, 32]); aff = sb("aff", [128, 32]); selv = sb("selv", [128, 32])
        cmp4 = sb("cmp4", [128, 8, 4, 4]); cnt = sb("cnt", [128, 32]); m2 = sb("m2", [128, 32])
        gs = sb("gs", [128, 8]); gmx = sb("gmx", [128, 1]); gsel = sb("gsel", [128, 8])
        wv = sb("wv", [128, 32]); wsum = sb("wsum", [128, 1]); gates = sb("gates", [128, 32])
        gtb = sb("gtb", [32, 128], BF16)
        yo = [sb("yo%d" % i, [128, 1024]) for i in range(2)]
        P.dma(rw[:], k.d["router_w"], w=["rw"])
        P.dma(rb[:], k.d["router_b"].partition_broadcast(128), w=["rb"])
        P.dma(g2bc[:, 0, :], k.d["modd"][l, 0:1, 5120:6144].partition_broadcast(128), w=["g2bc"])
        P.dma(g2bc[:, 1, :], k.d["modd"][l, 1:2, 5120:6144].partition_broadcast(128), w=["g2bc"])
        if final:
            P.dma(fnbc[:], k.d["fnorm"].partition_broadcast(128), w=["fnbc"])
        wg_d, wu_d, wd_d = k.d["moe_wg"], k.d["moe_wu"], k.d["moe_wd"]
        wit = 0
        sit = 0
        for blk in blocks:
            nb = len(blk)
            ntok = nb * 128
            for ti, i in enumerate(blk):
                b = i % 2
                x, kx = xt[b], "xt%d" % b
                P.dma(x[:], k.d["X"][i * 128:(i + 1) * 128, :], r=["X%d" % i], w=[kx])
                j = 0 if i >= 2 else 1
                hh, kh = h32[b], "h32_%d" % b
                norm_tile(k, x[:], kx, k.cols[:, l, 2, :, j], k.cols[:, l, 3, :, j], hh, kh)
                cp(P, "act", h2T[:, :, ti * 128:(ti + 1) * 128], hh[:], r=[kh], w=["h2T"])
                for c in range(8):
                    mm(P, ps[2][:, 0:32], hh[:, c, :], rw[:, c, :], start=(c == 0), stop=(c == 7), r=[kh, "rw"], w=["ps2"])
                actf(P, aff[:], ps[2][:, 0:32], AF.Sigmoid, r=["ps2"], w=["aff"])
                tt(P, "dve", selv[:], aff[:], rb[:], ALU.add, r=["aff", "rb"], w=["selv"])
                s3 = selv[:].rearrange("p (g e) -> p g e", e=4)
                tt(P, "dve", cmp4[:], bc(s3.unsqueeze(2), [128, 8, 4, 4]), bc(s3.unsqueeze(3), [128, 8, 4, 4]), ALU.is_gt,
                   r=["selv"], w=["cmp4"])
                red(P, "dve", cnt[:], cmp4[:].rearrange("p g i j -> p (g i) j"), ALU.add, r=["cmp4"], w=["cnt"])
                P.dve(lambda e: e.tensor_single_scalar(m2[:], cnt[:], 1.5, ALU.is_lt), r=["cnt"], w=["m2"])
                tt(P, "dve", wv[:], selv[:], m2[:], ALU.mult, r=["selv", "m2"], w=["wv"])
                red(P, "dve", gs[:], wv[:].rearrange("p (g e) -> p g e", e=4), ALU.add, r=["wv"], w=["gs"])
                red(P, "dve", gmx[:], gs[:], ALU.max, r=["gs"], w=["gmx"])
                ts(P, "dve", gsel[:], gs[:], gmx[:, 0:1], None, ALU.is_ge, r=["gs"], sr=["gmx"], w=["gsel"])
                tt(P, "dve", m2[:].rearrange("p (g e) -> p g e", e=4), m2[:].rearrange("p (g e) -> p g e", e=4),
                   bc(gsel[:].unsqueeze(2), [128, 8, 4]), ALU.mult, r=["m2", "gsel"], w=["m2"])
                tt(P, "dve", wv[:], aff[:], m2[:], ALU.mult, r=["aff", "m2"], w=["wv"])
                red(P, "dve", wsum[:], wv[:], ALU.add, r=["wv"], w=["wsum"])
                P.dve(lambda e: e.reciprocal(wsum[:], wsum[:]), r=["wsum"], w=["wsum"])
                ts(P, "dve", gates[:], wv[:], wsum[:, 0:1], None, ALU.mult, r=["wv"], sr=["wsum"], w=["gates"])
                if k.debug and ("gates%d" % l) in k.d:
                    P.dma(k.d["gates%d" % l][i * 128:(i + 1) * 128, :], gates[:], r=["gates"])
                if k.debug and "dbgA" in k.d:
                    P.dma(k.d["dbgA"][i * 128:(i + 1) * 128, :], aff[:], r=["aff"])
                    P.dma(k.d["dbgB"][i * 128:(i + 1) * 128, :], selv[:], r=["selv"])
                    P.dma(k.d["dbgC"][i * 128:(i + 1) * 128, :], m2[:], r=["m2"])
                    P.dma(k.d["dbgD"][i * 128:(i + 1) * 128, :], gs[:], r=["gs"])
                    P.dma(k.d["dbgE"][i * 128:(i + 1) * 128, :], cnt[:], r=["cnt"])
                tr(P, ps[3][0:32, 0:128], gates[:], k.identf[:], r=["gates", "identf"], w=["ps3"])
                cp(P, "act", gT[:, ti * 128:(ti + 1) * 128], ps[3][0:32, 0:128], r=["ps3"], w=["gT"])
            subs = [(s0, min(256, ntok - s0)) for s0 in range(0, ntok, 256)]
            dsubs = [(s0, min(512, ntok - s0)) for s0 in range(0, ntok, 512)]
            for eg in range(8):
                for el in range(4):
                    e_ = eg * 4 + el
                    wb = wgu[wit % 3]; kwb = "wgu%d" % (wit % 3); wit += 1
                    slot = (eg % 2) * 4 + el
                    wdd, kwd = wd[slot], "wd%d" % slot
                    Gs, kG = G[slot], "G%d" % slot
                    P.dma(wb[:], k.d["wgu_bf"][l, e_], w=[kwb])
                    P.dma(wdd[:], k.d["wd_bf"][l, e_], w=[kwd])
                    for si, (s0, sn) in enumerate(subs):
                        par = sit % 2; sit += 1
                        sgb, ksg = sg[par], "sg%d" % par
                        gb_, kgb = gbs[par], "gbs%d" % par
                        gbp = ps[2][:, par * 256:par * 256 + sn]; kgbp = "ps2_%d" % par
                        gp = ps[3 + par][:].rearrange("p (c t) -> p c t", t=256); kgp = "ps%d" % (3 + par)
                        up = ps[5 + par][:].rearrange("p (c t) -> p c t", t=256); kup = "ps%d" % (5 + par)
                        mm(P, gbp, k.sel[:, e_, :], gT[:, s0:s0 + sn], r=["sel", "gT"], w=["ps2"])
                        for c2 in range(2):
                            for c in range(8):
                                mm(P, gp[:, c2, 0:sn], wb[:, c, c2 * 128:(c2 + 1) * 128], h2T[:, c, s0:s0 + sn],
                                   start=(c == 0), stop=(c == 7), r=[kwb, "h2T"], w=[kgp])
                        for c2 in range(2):
                            for c in range(8):
                                mm(P, up[:, c2, 0:sn], wb[:, c, 256 + c2 * 128:256 + (c2 + 1) * 128], h2T[:, c, s0:s0 + sn],
                                   start=(c == 0), stop=(c == 7), r=[kwb, "h2T"], w=[kup])
                        cp(P, "act", gb_[:, 0:sn], gbp, r=["ps2"], w=[kgb])
                        actf(P, sgb[:, :, 0:sn], gp[:, :, 0:sn], AF.Silu, r=[kgp], w=[ksg])
                        tt(P, "pool", sgb[:, :, 0:sn], sgb[:, :, 0:sn], bc(gb_[:, 0:sn].unsqueeze(1), [128, 2, sn]), ALU.mult,
                           r=[ksg, kgb], w=[ksg])
                        tt(P, "dve", Gs[:, :, s0:s0 + sn], up[:, :, 0:sn], sgb[:, :, 0:sn], ALU.mult, r=[kup, ksg], w=[kG])
                for si, (s0, sn) in enumerate(dsubs):
                    for dc in range(8):
                        pb_ = ps[dc % 2]; kpb = "ps%d" % (dc % 2)
                        n = 0
                        for el in range(4):
                            slot = (eg % 2) * 4 + el
                            for c2 in range(2):
                                mm(P, pb_[:, 0:sn], wd[slot][:, c2, dc * 128:(dc + 1) * 128], G[slot][:, c2, s0:s0 + sn],
                                   start=(n == 0), stop=(n == 7), r=["wd%d" % slot, "G%d" % slot], w=[kpb])
                                n += 1
                        if eg == 0:
                            cp(P, "dve", acc[:, dc, s0:s0 + sn], pb_[:, 0:sn], r=[kpb], w=["acc"])
                        else:
                            tt(P, "dve", acc[:, dc, s0:s0 + sn], acc[:, dc, s0:s0 + sn], pb_[:, 0:sn], ALU.add,
                               r=[kpb, "acc"], w=["acc"])
            for ti, i in enumerate(blk):
                b = i % 2
                x, kx = xt[b], "xt%d" % b
                y_, ky = yo[b], "yo%d" % b
                P.dma(x[:], k.d["X"][i * 128:(i + 1) * 128, :], r=["X%d" % i], w=[kx])
                for c in range(8):
                    pp = ps[2 + c // 4]
                    tr(P, pp[:, (c % 4) * 128:(c % 4 + 1) * 128], acc[:, c, ti * 128:(ti + 1) * 128], k.identf[:],
                       r=["acc", "identf"], w=["ps%d" % (2 + c // 4)])
                j = 0 if i >= 2 else 1
                if k.debug and ("ffn%d" % l) in k.d:
                    for hf in range(2):
                        cp(P, "act", y_[:, hf * 512:(hf + 1) * 512], ps[2 + hf][:], r=["ps%d" % (2 + hf)], w=[ky])
                    P.dma(k.d["ffn%d" % l][i * 128:(i + 1) * 128, :], y_[:], r=[ky])
                for hf in range(2):
                    tt(P, "dve", y_[:, hf * 512:(hf + 1) * 512], ps[2 + hf][:], g2bc[:, j, hf * 512:(hf + 1) * 512], ALU.mult,
                       r=["ps%d" % (2 + hf), "g2bc"], w=[ky])
                tt(P, "pool", x[:], x[:], y_[:], ALU.add, r=[kx, ky], w=[kx])
                if not final:
                    P.dma(k.d["X"][i * 128:(i + 1) * 128, :], x[:], r=[kx], w=["X%d" % i])
                else:
                    ss2 = k.nss
                    actf(P, y_[:], x[:], AF.Square, r=[kx], w=[ky])
                    red(P, "dve", ss2[:], y_[:], ALU.add, r=[ky], w=["nss"])
                    ts(P, "dve", ss2[:], ss2[:], 1.0 / D, EPS, ALU.mult, ALU.add, r=["nss"], w=["nss"])
                    actf(P, ss2[:], ss2[:], AF.Sqrt, r=["nss"], w=["nss"])
                    P.dve(lambda e: e.reciprocal(ss2[:], ss2[:]), r=["nss"], w=["nss"])
                    stt(P, "dve", y_[:], x[:], ss2[:, 0:1], fnbc[:], ALU.mult, ALU.mult, r=[kx, "fnbc"], sr=["nss"], w=[ky])
                    P.dma(k.d["out"][(i - 2) * 128:(i - 1) * 128, :], y_[:], r=[ky])
        P.emit()


INPUT_SPECS = {
    "xin": ([T, D], F32), "csv": ([128, 8, 2], F32), "ada_w": ([2, D, 6 * D], F32), "ada_b": ([2, 6 * D], F32),
    "nmix": ([128, 2, 8], F32), "nffn": ([128, 2, 8], F32), "fnorm": ([1, D], F32),
    "w_in0": ([D, 2080], F32), "w_out0": ([D, D], F32), "a2blk": ([2, 33, 256], F32), "gnorm0": ([1, 512], F32),
    "s5_lam": ([64, 2, 2, 32], F32), "s5_logdt": ([1, 64], F32), "s5_b": ([64, 2, 2, 32, 16], F32),
    "s5_c": ([64, 2, 2, 32, 16], F32), "s5_d": ([1, 512], F32), "glu_w": ([512, 512], F32), "glu_b": ([128, 4], F32),
    "hg_w_in": ([D, 5 * D], F32), "hg_w_out": ([D, D], F32), "hg_lbrow": ([2, D], F32), "hg_lbcol": ([128, 2, 8], F32),
    "gnorm1": ([1, 512], F32),
    "router_w": ([128, 8, 32], F32), "router_b": ([1, 32], F32),
    "moe_wg": ([2, 32, D, 256], F32), "moe_wu": ([2, 32, D, 256], F32), "moe_wd": ([2, 32, 256, D], F32),
}
SCRATCH_SPECS = {
    "modd": ([2, 2, 6 * D], F32), "t64d": ([64, 512], F32), "X": ([T, D], F32), "hT": ([128, 8, T], BF16),
    "of": ([T, D], F32), "s5oT": ([128, 4, T], BF16), "u_d": ([T, 512], F32),
    "wgu_bf": ([2, 32, 128, 8, 512], BF16), "wd_bf": ([2, 32, 128, 2, 1024], BF16),
}


def build(stages, debug=False, ext_in=(), ext_out=(), dbg=None):
    nc = bass.Bass("TRN2", target_bir_lowering=False)
    k = K()
    k.nc = nc
    k.debug = debug
    k.d = {}
    for name, (shape, dt) in INPUT_SPECS.items():
        k.d[name] = nc.dram_tensor(name, shape, dt, kind="ExternalInput").ap()
    for name, (shape, dt) in SCRATCH_SPECS.items():
        kind = "ExternalInput" if name in ext_in else ("ExternalOutput" if name in ext_out else "Internal")
        k.d[name] = nc.dram_tensor(name, shape, dt, kind=kind).ap()
    for name, (shape, dt) in (dbg or {}).items():
        k.d[name] = nc.dram_tensor(name, shape, dt, kind="ExternalOutput").ap()
    k.d["out"] = nc.dram_tensor("out", [NLAT, D], F32, kind="ExternalOutput").ap()
    with contextlib.ExitStack() as es0:
        k.P = Prog(nc, es0)
        k.sbp = _sb(nc, es0)
        k.ps = [es0.enter_context(nc.psum_tensor("ps%d" % i, [128, 512], F32)) for i in range(7)]
        k.psb = es0.enter_context(nc.psum_tensor("psb", [128, 1024], BF16))
        alloc_consts(k)
        for st in stages:
            if st == "pro":
                stage_prologue(k)
            elif st == "consts_only":
                k.P.emit()
            elif st[0] == "A":
                stage_A(k, int(st[1]))
            elif st[0] == "B":
                stage_AC(k, int(st[1]))
            elif st[0] == "M":
                stage_moe(k, int(st[1]), final=(st[1] == "1"))
            elif st == "S5":
                stage_s5(k)
            elif st[0] == "C":
                stage_precast(k, int(st[1]))
            elif st[0] == "R":
                stage_rec(k, int(st[1]))
            else:
                raise ValueError(st)
    return nc


def host_inputs(inp, b):
    f = lambda a: np.ascontiguousarray(np.asarray(a, dtype=np.float32))
    col = lambda v: f(np.asarray(v).reshape(8, 128).T)
    m = {}
    m["xin"] = f(np.concatenate([inp["ctx"][b], inp["x"][b]], axis=0))
    m["csv"] = f(np.stack([col(inp["c"][b]), col(inp["c_ctx"])], axis=-1))
    m["ada_w"] = f(inp["ada_w"]); m["ada_b"] = f(inp["ada_b"])
    m["nmix"] = f(np.stack([col(inp["norm_mix"][l]) for l in range(2)], axis=1))
    m["nffn"] = f(np.stack([col(inp["norm_ffn"][l]) for l in range(2)], axis=1))
    m["fnorm"] = f(inp["final_norm"].reshape(1, D))
    m["w_in0"] = f(inp["ab_w_in"][0]); m["w_out0"] = f(inp["ab_w_out"][0])
    a2 = np.zeros((2, 33, 256), np.float32)
    a2[0, 0:16] = inp["gla_a2"][0, 0]; a2[1, 16:32] = inp["gla_a2"][0, 1]
    a2[0, 32] = inp["gla_ab"][0, 0]; a2[1, 32] = inp["gla_ab"][0, 1]
    m["a2blk"] = a2
    m["gnorm0"] = f(np.tile(inp["gla_norm"][0], 4).reshape(1, 512))
    m["s5_lam"] = f(np.stack([inp["s5_lam_re"][0], inp["s5_lam_im"][0]], 0).transpose(3, 0, 1, 2))
    m["s5_logdt"] = f(inp["s5_log_dt"][0].reshape(1, 64))
    m["s5_b"] = f(np.stack([inp["s5_b_re"][0], inp["s5_b_im"][0]], 0).transpose(3, 0, 1, 2, 4))
    m["s5_c"] = f(np.stack([inp["s5_c_re"][0], inp["s5_c_im"][0]], 0).transpose(4, 0, 1, 2, 3))
    m["s5_d"] = f(inp["s5_d"][0].reshape(1, 512))
    m["glu_w"] = f(inp["s5_glu_w"][0]); m["glu_b"] = f(inp["s5_glu_b"][0].reshape(4, 128).T)
    m["hg_w_in"] = f(inp["hg_w_in"][0]); m["hg_w_out"] = f(inp["hg_w_out"][0])
    m["hg_lbrow"] = f(inp["hg_lb_logits"]); m["hg_lbcol"] = f(np.stack([col(inp["hg_lb_logits"][l]) for l in range(2)], axis=1))
    m["gnorm1"] = f(np.tile(inp["hg_norm"][0], 4).reshape(1, 512))
    m["router_w"] = f(inp["router_w"].reshape(8, 128, 32).transpose(1, 0, 2))
    m["router_b"] = f(inp["router_bias"].reshape(1, 32))
    m["moe_wg"] = f(inp["moe_w_gate"]); m["moe_wu"] = f(inp["moe_w_up"]); m["moe_wd"] = f(inp["moe_w_down"])
    return m


ALL_STAGES = ["pro", "B0", "S5", "R0", "M0", "B1", "R1", "M1"]


def kernel(**inputs):
    nc = build(ALL_STAGES)
    shared = None
    in_maps = []
    for b in range(8):
        m = host_inputs(inputs, b)
        if shared is None:
            shared = m
        else:
            for kk in m:
                if kk not in ("xin", "csv"):
                    m[kk] = shared[kk]
        in_maps.append(m)
    res = run_bass_kernel_spmd(nc, in_maps, core_ids=list(range(8)))
    return np.stack([np.asarray(r["out"], dtype=np.float32) for r in res.results], axis=0)


def stage_rec(k, l):
    nc, P, ps, psb = k.nc, k.P, k.ps, k.psb
    gla = (l == 0)
    NG = 1 if gla else 2
    J = 2 if gla else 4
    dk = 64 if gla else 128
    NW = 1568 if gla else 5120
    heads = []
    for h in range(4):
        heads.append((h // 2, (h % 2) * 64) if gla else (h, 0))
    win_d = k.d["w_in0"] if gla else k.d["hg_w_in"]
    wout_d = k.d["w_out0"] if gla else k.d["hg_w_out"]
    for di in range(2):
        with contextlib.ExitStack() as es:
            sb = _sb(nc, es)
            W = sb("W", [128, 8, NW], BF16)
            P.dma(W[:], win_d[:, 0:NW].rearrange("(c p) n -> p c n", p=128), w=["W"], q="pool")
            hT = [sb("hT%d" % i, [128, 8, 128], BF16) for i in range(2)]
            S = sb("S", [128, NG, J, 128]); Sbf = sb("Sbf", [128, NG, J, 128], BF16)
            P.pool(lambda e: e.memset(S[:], 0.0), w=["S"])
            P.pool(lambda e: e.memset(Sbf[:], 0.0), w=["Sbf"])
            qT = sb("qT", [128, J, 128]); kT = sb("kT", [128, J, 128]); la = sb("la", [128, J, 128])
            Bp = sb("Bp", [128, J, 128]); Bt = sb("Bt", [128, J, 128]); D1 = sb("D1", [128, J, 128]); D2 = sb("D2", [128, J, 128])
            eq = sb("eq", [128, J, 128]); ek = sb("ek", [128, J, 128]); ei = sb("ei", [128, J, 128]); ekd = sb("ekd", [128, J, 128])
            dec = sb("dec", [128, J, 2])
            qs = sb("qs", [128, J, 128], BF16); ks_ = sb("ks", [128, J, 128], BF16); qi = sb("qi", [128, J, 128], BF16)
            kdT = sb("kdT", [128, J, 128], BF16); kd = sb("kd", [128, J * 128], BF16)
            vbf = sb("vbf", [128, 512], BF16); attn = sb("attn", [128, 4, 128], BF16)
            osb = [sb("osb%d" % i, [128, 512]) for i in range(2)]
            if gla:
                hm2 = sb("hm2", [128, 2]); qsm = sb("qsm", [128, 4, 128], BF16); qim = sb("qim", [128, 4, 128], BF16)
                P.pool(lambda e: e.memset(hm2[:], 0.0), w=["hm2"])
                P.pool(lambda e: e.memset(hm2[0:64, 0:1], 1.0), r=["hm2"], w=["hm2"])
                P.pool(lambda e: e.memset(hm2[64:128, 1:2], 1.0), r=["hm2"], w=["hm2"])
                a2 = sb("a2", [33, 256]); afbT = sb("afbT", [33, 128])
                P.dma(a2[:], k.d["a2blk"][di], w=["a2"])
                P.pool(lambda e: e.memset(afbT[32:33, :], 1.0), w=["afbT"])
            else:
                lbc = sb("lbc", [128, 2, 8]); lb = sb("lb", [128, 8]); oml = sb("oml", [128, 8]); sgm = sb("sgm", [128, J, 128])
                P.dma(lbc[:], k.d["hg_lbcol"], w=["lbc"])
                tt(P, "dve", lb[:], lbc[:, 1, :], lbc[:, 0, :], ALU.subtract, r=["lbc"], w=["lb"])
                actf(P, lb[:], lb[:], AF.Sigmoid, r=["lb"], w=["lb"])
                ts(P, "dve", oml[:], lb[:], -1.0, 1.0, ALU.mult, ALU.add, r=["lb"], w=["oml"])
            if di == 1:
                wout = sb("wout", [128, 8, 1024], BF16)
                P.dma(wout[:], wout_d.rearrange("(c p) n -> p c n", p=128), w=["wout"], q="pool")
                gn = sb("gn", [128, 512]); g1bc = sb("g1bc", [128, 2, 1024])
                P.dma(gn[:], k.d["gnorm%d" % l].partition_broadcast(128), w=["gn"])
                P.dma(g1bc[:, 0, :], k.d["modd"][l, 0:1, 2048:3072].partition_broadcast(128), w=["g1bc"])
                P.dma(g1bc[:, 1, :], k.d["modd"][l, 1:2, 2048:3072].partition_broadcast(128), w=["g1bc"])
                oft = [sb("oft%d" % i, [128, 512]) for i in range(2)]
                o32 = sb("o32", [128, 4, 128]); sq = sb("sq", [128, 4, 128]); ssq = sb("ssq", [128, 4])
                sgl = sb("sgl", [128, 512]); mix = sb("mix", [128, 512], BF16)
                mixT = [sb("mixT%d" % i, [128, 8, 128], BF16) for i in range(2)]
                xt = [sb("xt%d" % i, [128, 1024]) for i in range(2)]
                yt = sb("yt", [128, 1024])
            order = list(range(NT)) if di == 0 else [1, 0] + list(range(NT - 1, 1, -1))
            mask = k.maskf if di == 0 else k.maskb
            kmask = "maskf" if di == 0 else "maskb"
            mid = 31 if di == 0 else 32
            end = 63 if di == 0 else 0
            v4 = lambda ap: ap.rearrange("p j (c t) -> p j c t", t=64)
            qiH = [sb("qiH%d" % i, [128, 4, 128], BF16) for i in range(2)]
            kdH = [sb("kdH%d" % i, [128, J * 128], BF16) for i in range(2)]
            vH = [sb("vH%d" % i, [128, 512], BF16) for i in range(2)]
            atH = [sb("atH%d" % i, [128, 4, 128], BF16) for i in range(2)]
            dcH = [sb("dcH%d" % i, [128, J, 2]) for i in range(2)]
            items = [(it, i, gi) for it, i in enumerate(order) for gi in range(NG)]
            p0 = ps[0][:].rearrange("p (j t) -> p j t", t=128)
            p1 = ps[1][:].rearrange("p (j t) -> p j t", t=128)
            p2 = ps[2][:].rearrange("p (j t) -> p j t", t=128)
            a_ps = ps[4][:].rearrange("p (h t) -> p h t", t=128)
            o_ps = ps[5][:].rearrange("p (h t) -> p h t", t=128)
            d_ps = ps[6][:].rearrange("p (j t) -> p j t", t=128)

            def front(n):
                it, i, gi = items[n]
                b = it % 2
                pn = n % 2
                h_, kh = hT[b], "hT%d" % b
                off = gi * 512
                if gi == 0:
                    P.dma(h_[:], k.d["hT"][:, :, i * 128:(i + 1) * 128], w=[kh])
                if gla:
                    for cc in range(4):
                        for c in range(8):
                            mm(P, p0[:, cc, :], W[:, c, cc * 128:(cc + 1) * 128], h_[:, c, :], start=(c == 0), stop=(c == 7),
                               r=["W", kh], w=["ps0"])
                    for c in range(8):
                        mm(P, ps[1][0:32, 0:128], W[:, c, 1536:1568], h_[:, c, :], start=(c == 0), stop=(c == 7),
                           r=["W", kh], w=["ps1"])
                    P.act(lambda e: e.mul(qT[:], p0[:, 0:2, :], 0.125), r=["ps0"], w=["qT"])
                    cp(P, "act", kT[:], p0[:, 2:4, :], r=["ps0"], w=["kT"])
                    cp(P, "dve", afbT[0:32, :], ps[1][0:32, 0:128], r=["ps1"], w=["afbT"])
                    for j in range(2):
                        mm(P, p2[:, j, :], a2[:, j * 128:(j + 1) * 128], afbT[:], r=["a2", "afbT"], w=["ps2"])
                    actf(P, la[:], p2[:, 0:2, :], AF.Exp, r=["ps2"], w=["la"], scale=-1.0)
                    actf(P, la[:], la[:], AF.Ln, r=["la"], w=["la"], bias=1.0)
                    ts(P, "dve", la[:], la[:], -1.0 / 16.0, None, ALU.mult, r=["la"], w=["la"])
                    vcol = 512
                else:
                    fcol = 1024 if di == 0 else 2048
                    for j in range(4):
                        for c in range(8):
                            mm(P, p0[:, j, :], W[:, c, off + j * 128: off + (j + 1) * 128], h_[:, c, :], start=(c == 0), stop=(c == 7),
                               r=["W", kh], w=["ps0"])
                    for j in range(4):
                        for c in range(8):
                            mm(P, p1[:, j, :], W[:, c, fcol + off + j * 128: fcol + off + (j + 1) * 128], h_[:, c, :],
                               start=(c == 0), stop=(c == 7), r=["W", kh], w=["ps1"])
                    actf(P, qT[:], p0, AF.Silu, r=["ps0"], w=["qT"])
                    actf(P, sgm[:], p1, AF.Sigmoid, r=["ps1"], w=["sgm"])
                    tt(P, "dve", sgm[:], sgm[:], bc(oml[:, gi * 4:gi * 4 + 4].unsqueeze(2), [128, 4, 128]), ALU.mult,
                       r=["sgm", "oml"], w=["sgm"])
                    tt(P, "dve", sgm[:], sgm[:], bc(lb[:, gi * 4:gi * 4 + 4].unsqueeze(2), [128, 4, 128]), ALU.add,
                       r=["sgm", "lb"], w=["sgm"])
                    ts(P, "dve", kT[:], sgm[:], -1.0, 1.0, ALU.mult, ALU.add, r=["sgm"], w=["kT"])
                    actf(P, la[:], sgm[:], AF.Ln, r=["sgm"], w=["la"])
                    vcol = 3072 + off
                vbf, kv = vH[pn], "vH%d" % pn
                for c in range(8):
                    mm(P, ps[3][:], h_[:, c, :], W[:, c, vcol:vcol + 512], start=(c == 0), stop=(c == 7), r=["W", kh], w=["ps3"])
                cp(P, "act", vbf[:], ps[3][:], r=["ps3"], w=[kv])
                for j in range(J):
                    P.dve(lambda e, j=j: e.tensor_tensor_scan(Bp[:, j, :], k.scanm[:], la[:, j, :], 0.0, ALU.mult, ALU.add),
                          r=["scanm", "la"], w=["Bp"])
                if di == 0:
                    Bq, kB = Bp, "Bp"
                else:
                    tt(P, "dve", Bt[:], la[:], Bp[:], ALU.subtract, r=["la", "Bp"], w=["Bt"])
                    tt(P, "dve", v4(Bt[:]), v4(Bt[:]), bc(v4(Bp[:])[:, :, :, 63:64], [128, J, 2, 64]), ALU.add, r=["Bt", "Bp"], w=["Bt"])
                    Bq, kB = Bt, "Bt"
                tt(P, "dve", v4(D1[:]), v4(Bq[:]), bc(v4(Bq[:])[:, :, :, mid:mid + 1], [128, J, 2, 64]), ALU.subtract, r=[kB], w=["D1"])
                tt(P, "pool", v4(D2[:]), bc(v4(Bq[:])[:, :, :, end:end + 1], [128, J, 2, 64]), v4(Bq[:]), ALU.subtract, r=[kB], w=["D2"])
                actf(P, eq[:], D1[:], AF.Exp, r=["D1"], w=["eq"])
                actf(P, ek[:], D1[:], AF.Exp, r=["D1"], w=["ek"], scale=-1.0)
                actf(P, ei[:], Bq[:], AF.Exp, r=[kB], w=["ei"])
                actf(P, ekd[:], D2[:], AF.Exp, r=["D2"], w=["ekd"])
                actf(P, dcH[pn][:], v4(Bq[:])[:, :, :, end], AF.Exp, r=[kB], w=["dcH%d" % pn])
                tt(P, "dve", qs[:], qT[:], eq[:], ALU.mult, r=["qT", "eq"], w=["qs"])
                tt(P, "pool", ks_[:], kT[:], ek[:], ALU.mult, r=["kT", "ek"], w=["ks"])
                tt(P, "dve", qi[:], qT[:], ei[:], ALU.mult, r=["qT", "ei"], w=["qi"])
                tt(P, "pool", kdT[:], kT[:], ekd[:], ALU.mult, r=["kT", "ekd"], w=["kdT"])
                for j in range(J):
                    tr(P, psb[:, j * 128:(j + 1) * 128], kdT[:, j, :], k.identb[:], r=["kdT", "identb"], w=["psb"])
                cp(P, "act", kdH[pn][:], psb[:, 0:J * 128], r=["psb"], w=["kdH%d" % pn])
                if gla:
                    hmb = bc(hm2[:].unsqueeze(1).unsqueeze(3), [128, 2, 2, 128])
                    tt(P, "pool", qsm[:].rearrange("p (j f) t -> p j f t", f=2), bc(qs[:].unsqueeze(2), [128, 2, 2, 128]), hmb, ALU.mult,
                       r=["qs", "hm2"], w=["qsm"])
                    tt(P, "pool", qiH[pn][:].rearrange("p (j f) t -> p j f t", f=2), bc(qi[:].unsqueeze(2), [128, 2, 2, 128]), hmb, ALU.mult,
                       r=["qi", "hm2"], w=["qiH%d" % pn])
                    qs_h, kqs = qsm, "qsm"
                else:
                    cp(P, "pool", qiH[pn][:], qi[:], r=["qi"], w=["qiH%d" % pn])
                    qs_h, kqs = qs, "qs"
                for h, (j, pb) in enumerate(heads):
                    mm(P, a_ps[:, h, :], ks_[:, j, :], qs_h[:, h, :], r=["ks", kqs], w=["ps4"])
                tt(P, "dve", atH[pn][:], a_ps, bc(mask[:].unsqueeze(1), [128, 4, 128]), ALU.mult, r=["ps4", kmask], w=["atH%d" % pn])

            def chain(n):
                it, i, gi = items[n]
                b = it % 2
                pn = n % 2
                h_, kh = hT[b], "hT%d" % b
                off = gi * 512
                lat = i >= 2
                need_out = gla or lat
                vbf, kv = vH[pn], "vH%d" % pn
                attn, kat = atH[pn], "atH%d" % pn
                kd, kkd = kdH[pn], "kdH%d" % pn
                qi_h, kqi = qiH[pn], "qiH%d" % pn
                dec, kdec = dcH[pn], "dcH%d" % pn
                gcol = 1024 if gla else 4096 + off
                for h in range(4):
                    mm(P, o_ps[:, h, :], attn[:, h, :], vbf[:, h * 128:(h + 1) * 128], start=(h == 0), stop=False,
                       r=[kat, kv], w=["ps5"])
                for ci, r0 in enumerate((0, 64) if di == 0 else (64, 0)):
                    cidx = r0 // 64
                    for h, (j, pb) in enumerate(heads):
                        mm(P, o_ps[r0:r0 + 64, h, :], qi_h[:, h, r0:r0 + 64], Sbf[:, gi, j, :], start=False, stop=True,
                           r=[kqi, "Sbf"], w=["ps5"])
                    for h, (j, pb) in enumerate(heads):
                        mm(P, d_ps[pb:pb + dk, j, :], kd[r0:r0 + 64, j * 128 + pb:j * 128 + pb + dk], vbf[r0:r0 + 64, h * 128:(h + 1) * 128],
                           r=[kkd, kv], w=["ps6"])
                    tt(P, "dve", S[:, gi], S[:, gi], bc(dec[:, :, cidx:cidx + 1], [128, J, 128]), ALU.mult, r=["S", kdec], w=["S"])
                    tt(P, "dve", S[:, gi], S[:, gi], d_ps[:, 0:J, :], ALU.add, r=["S", "ps6"], w=["S"])
                    cp(P, "act", Sbf[:, gi], S[:, gi], r=["S"], w=["Sbf"])
                if not need_out:
                    return
                if di == 0:
                    ob, kob = osb[n % 2], "osb%d" % (n % 2)
                    cp(P, "act", ob[:], ps[5][:], r=["ps5"], w=[kob])
                    P.dma(k.d["of"][i * 128:(i + 1) * 128, off:off + 512], ob[:], r=[kob], w=["of%d_%d" % (i, gi)])
                    return
                ofb, kofb = oft[n % 2], "oft%d" % (n % 2)
                P.dma(ofb[:], k.d["of"][i * 128:(i + 1) * 128, off:off + 512], w=[kofb])
                o3 = o32[:].rearrange("p h t -> p (h t)")
                tt(P, "dve", o3, ps[5][:], ofb[:], ALU.add, r=["ps5", kofb], w=["o32"])
                if k.debug and ("dbg_o%d" % l) in k.d:
                    P.dma(k.d["dbg_o%d" % l][i * 128:(i + 1) * 128, off:off + 512], o3, r=["o32"])
                tt(P, "pool", sq[:], o32[:], o32[:], ALU.mult, r=["o32"], w=["sq"])
                red(P, "dve", ssq[:], sq[:], ALU.add, r=["sq"], w=["ssq"])
                ts(P, "dve", ssq[:], ssq[:], 1.0 / 128, EPS, ALU.mult, ALU.add, r=["ssq"], w=["ssq"])
                actf(P, ssq[:], ssq[:], AF.Sqrt, r=["ssq"], w=["ssq"])
                P.dve(lambda e: e.reciprocal(ssq[:], ssq[:]), r=["ssq"], w=["ssq"])
                tt(P, "dve", o32[:], o32[:], bc(ssq[:].unsqueeze(2), [128, 4, 128]), ALU.mult, r=["o32", "ssq"], w=["o32"])
                tt(P, "pool", o3, o3, gn[:], ALU.mult, r=["o32", "gn"], w=["o32"])
                for c in range(8):
                    mm(P, ps[6][:], h_[:, c, :], W[:, c, gcol:gcol + 512], start=(c == 0), stop=(c == 7), r=["W", kh], w=["ps6"])
                actf(P, sgl[:], ps[6][:], AF.Silu, r=["ps6"], w=["sgl"])
                tt(P, "dve", mix[:], o3, sgl[:], ALU.mult, r=["o32", "sgl"], w=["mix"])
                mT, kmT = mixT[b], "mixT%d" % b
                for c in range(4):
                    tr(P, psb[:, 512 + c * 128:512 + (c + 1) * 128], mix[:, c * 128:(c + 1) * 128], k.identb[:], r=["mix", "identb"], w=["psb"])
                cp(P, "act", mT[:, gi * 4:gi * 4 + 4, :], psb[:, 512:1024].rearrange("p (c t) -> p c t", t=128), r=["psb"], w=[kmT])
                if gi != NG - 1:
                    return
                if gla:
                    P.dma(mT[:, 4:8, :], k.d["s5oT"][:, :, i * 128:(i + 1) * 128], w=[kmT])
                x, kx = xt[b], "xt%d" % b
                P.dma(x[:], k.d["X"][i * 128:(i + 1) * 128, :], w=[kx])
                for hf in range(2):
                    for c in range(8):
                        mm(P, ps[5 + hf][:], mT[:, c, :], wout[:, c, hf * 512:(hf + 1) * 512], start=(c == 0), stop=(c == 7),
                           r=[kmT, "wout"], w=["ps%d" % (5 + hf)])
                jj = 0 if lat else 1
                for hf in range(2):
                    tt(P, "dve", yt[:, hf * 512:(hf + 1) * 512], ps[5 + hf][:], g1bc[:, jj, hf * 512:(hf + 1) * 512], ALU.mult,
                       r=["ps%d" % (5 + hf), "g1bc"], w=["yt"])
                tt(P, "pool", x[:], x[:], yt[:], ALU.add, r=[kx, "yt"], w=[kx])
                P.dma(k.d["X"][i * 128:(i + 1) * 128, :], x[:], r=[kx], w=["Xw%d" % i])

            prevC = None
            for n in range(len(items)):
                fa = P.capture(lambda: front(n))
                if prevC is None:
                    P.replay(fa)
                else:
                    P.merge(fa, prevC)
                prevC = P.capture(lambda: chain(n))
            P.replay(prevC)
            P.emit()


def stage_s5(k):
    nc, P, ps, psb = k.nc, k.P, k.ps, k.psb
    CT = [(256, 128, 0), (256 + 2048, 128, 128), (0, 16, 256)]
    TWO_PI = 2.0 * math.pi
    with contextlib.ExitStack() as es:
        sb = _sb(nc, es)
        Wu = sb("Wu", [128, 8, 512], BF16)
        P.dma(Wu[:], k.d["w_in0"][:, 1568:2080].rearrange("(c p) n -> p c n", p=128), w=["Wu"], q="pool")
        Uf = sb("Uf", [128, 3, 16, 128])
        Ug = sb("Ug", [128, 8, 2, 272], BF16)
        Yp = sb("Yp", [128, 3, 16, 128], BF16)
        nrow = sb("nrow", [64, 5, 16]); nio = sb("nio", [64, 16], I32)
        P.pool(lambda e: e.iota(nio[:], [[1, 16]], base=0, channel_multiplier=0), w=["nio"])
        cp(P, "dve", nrow[:, 0, :], nio[:], r=["nio"], w=["nrow"])
        ts(P, "dve", nrow[:, 1, :], nrow[:, 0, :], -1.0, None, ALU.mult, r=["nrow"], w=["nrow"])
        ts(P, "dve", nrow[:, 2, :], nrow[:, 0, :], 1.0, None, ALU.add, r=["nrow"], w=["nrow"])
        ts(P, "dve", nrow[:, 3, :], nrow[:, 0, :], -1.0, 15.0, ALU.mult, ALU.add, r=["nrow"], w=["nrow"])
        ts(P, "dve", nrow[:, 4, :], nrow[:, 0, :], -1.0, 16.0, ALU.mult, ALU.add, r=["nrow"], w=["nrow"])
        tio = sb("tio", [128, 256], I32); tf = sb("tf", [128, 256]); pio = sb("pio", [128, 1], I32); slo = sb("slo", [128, 1])
        mT = sb("mT", [128, 2, 2, 256]); mtmp = sb("mtmp", [128, 256])
        P.pool(lambda e: e.iota(tio[:].rearrange("p (t h) -> p t h", h=16), [[1, 16], [0, 16]], base=0, channel_multiplier=0), w=["tio"])
        P.pool(lambda e: e.iota(pio[:], [[0, 1]], base=0, channel_multiplier=1), w=["pio"])
        cp(P, "dve", tf[:], tio[:], r=["tio"], w=["tf"])
        P.dve(lambda e: e.tensor_single_scalar(pio[:], pio[:], 4, ALU.arith_shift_right), r=["pio"], w=["pio"])
        cp(P, "pool", slo[:], pio[:], r=["pio"], w=["slo"])
        for sh in range(2):
            ts(P, "dve", mT[:, 0, sh, :], tf[:], float(-8 * sh), slo[:, 0:1], ALU.add, ALU.is_ge, r=["tf"], sr=["slo"], w=["mT"])
            ts(P, "dve", mtmp[:], tf[:], -1.0, float(8 * sh), ALU.mult, ALU.add, r=["tf"], w=["mtmp"])
            ts(P, "dve", mT[:, 1, sh, :], mtmp[:], slo[:, 0:1], 0.0, ALU.add, ALU.is_ge, r=["mtmp"], sr=["slo"], w=["mT"])
        dbc = sb("dbc", [128, 512])
        P.dma(dbc[:], k.d["s5_d"].partition_broadcast(128), w=["dbc"])
        lam = sb("lam", [64, 2, 2, 8]); ctl = sb("ctl", [64, 2, 2, 8, 16])
        Tre = sb("Tre", [64, 16, 5, 16]); Tim = sb("Tim", [64, 16, 5, 16])
        Bre = sb("Bre", [64, 16, 16]); Bim = sb("Bim", [64, 16, 16])
        Ar2 = sb("Ar2", [64, 2, 2, 8]); AiS = sb("AiS", [64, 2, 2, 8])
        P.emit()
        for fc in range(4):
            esA = contextlib.ExitStack(); sbA = _sb(nc, esA)
            hTc = sbA("hTc", [128, 8, 2048], BF16); Ub = sbA("Ub", [128, 3, 8, 16, 16], BF16)
            for ct, (tb, M, col0) in enumerate(CT):
                P.dma(hTc[:, :, 0:M * 16], k.d["hT"][:, :, tb:tb + M * 16], w=["hTc"])
                for s in range(16):
                    pp = ps[s % 2]; kp = "ps%d" % (s % 2)
                    for c in range(8):
                        mm(P, pp[0:M, 0:128], hTc[:, c, s:M * 16:16], Wu[:, c, fc * 128:(fc + 1) * 128], start=(c == 0), stop=(c == 7),
                           r=["hTc", "Wu"], w=[kp])
                    cp(P, "act", Uf[0:M, ct, s, :], pp[0:M, 0:128], r=[kp], w=["Uf"])
                    cp(P, "dve", Ub[0:M, ct, :, s, :], pp[0:M, 0:128].rearrange("p (g h) -> p g h", h=16), r=[kp], w=["Ub"])
                if k.debug and "u_d" in k.d:
                    P.dma(k.d["u_d"][tb:tb + M * 16, fc * 128:(fc + 1) * 128].rearrange("(c s) n -> c s n", s=16), Uf[0:M, ct], r=["Uf"])
                for g0 in range(0, 8, 4):
                    for g in range(g0, g0 + 4):
                        for sh in range(2):
                            slot = (g - g0) * 2 + sh
                            tr(P, psb[:, slot * 128:slot * 128 + M], Ub[0:M, ct, g, sh * 8:(sh + 1) * 8, :].rearrange("p s h -> p (s h)"),
                               k.identb[0:M, 0:M], r=["Ub", "identb"], w=["psb"])
                    cp(P, "act", Ug[:, g0:g0 + 4, :, col0:col0 + M],
                       psb[:].rearrange("p (g s c) -> p g s c", g=4, s=2)[:, :, :, 0:M], r=["psb"], w=["Ug"])
            P.emit(); esA.close()
            esB = contextlib.ExitStack(); sbB = _sb(nc, esB)
            dtb = sbB("dtb", [64, 2, 8]); lr = sbB("lr", [64, 16]); li = sbB("li", [64, 16]); bt = sbB("bt", [64, 2, 2, 8, 16])
            are = sbB("are", [64, 16, 80]); ys = sbB("ys", [64, 16, 80]); yc = sbB("yc", [64, 16, 80])
            yi = sbB("yi", [64, 16, 80], I32); yf = sbB("yf", [64, 16, 80])
            den = sbB("den", [64, 16]); am1 = sbB("am1", [64, 16]); fre = sbB("fre", [64, 16]); fim = sbB("fim", [64, 16]); ftmp = sbB("ftmp", [64, 16])
            btmp = sbB("btmp", [64, 16, 16])
            P.dma(lam[:], k.d["s5_lam"][:, :, :, fc * 8:(fc + 1) * 8], w=["lam"])
            P.dma(dtb[:], k.d["s5_logdt"].rearrange("o (d g) -> o d g", d=2)[:, :, fc * 8:(fc + 1) * 8].partition_broadcast(64), w=["dtb"])
            P.dma(bt[:], k.d["s5_b"][:, :, :, fc * 8:(fc + 1) * 8, :], w=["bt"])
            P.dma(ctl[:], k.d["s5_c"][:, :, :, fc * 8:(fc + 1) * 8, :], w=["ctl"])
            actf(P, dtb[:], dtb[:], AF.Exp, r=["dtb"], w=["dtb"])
            lr3 = lr[:].rearrange("p (d g) -> p d g", d=2); li3 = li[:].rearrange("p (d g) -> p d g", d=2)
            tt(P, "dve", lr3, lam[:, 0], dtb[:], ALU.mult, r=["lam", "dtb"], w=["lr"])
            tt(P, "dve", li3, lam[:, 1], dtb[:], ALU.mult, r=["lam", "dtb"], w=["li"])
            nr = nrow[:].rearrange("p a b -> p (a b)")
            tt(P, "dve", are[:], bc(lr[:].unsqueeze(2), [64, 16, 80]), bc(nr.unsqueeze(1), [64, 16, 80]), ALU.mult, r=["lr", "nrow"], w=["are"])
            tt(P, "dve", ys[:], bc(li[:].unsqueeze(2), [64, 16, 80]), bc(nr.unsqueeze(1), [64, 16, 80]), ALU.mult, r=["li", "nrow"], w=["ys"])
            actf(P, are[:], are[:], AF.Exp, r=["are"], w=["are"])
            ts(P, "dve", yc[:], ys[:], 1.0 / TWO_PI, 0.75, ALU.mult, ALU.add, r=["ys"], w=["yc"])
            ts(P, "dve", ys[:], ys[:], 1.0 / TWO_PI, 0.5, ALU.mult, ALU.add, r=["ys"], w=["ys"])
            sin_reduced(P, ys[:], ys[:], yi[:], yf[:], "ys", "yi", "yf", "ys")
            sin_reduced(P, yc[:], yc[:], yi[:], yf[:], "yc", "yi", "yf", "yc")
            Tre3 = Tre[:].rearrange("p q a b -> p q (a b)"); Tim3 = Tim[:].rearrange("p q a b -> p q (a b)")
            tt(P, "dve", Tre3, are[:], yc[:], ALU.mult, r=["are", "yc"], w=["Tre"])
            tt(P, "dve", Tim3, are[:], ys[:], ALU.mult, r=["are", "ys"], w=["Tim"])
            lre = lam[:, 0].rearrange("p d g -> p (d g)"); lim = lam[:, 1].rearrange("p d g -> p (d g)")
            a_re = Tre[:, :, 2, 0]; a_im = Tim[:, :, 2, 0]
            tt(P, "dve", den[:], lre, lre, ALU.mult, r=["lam"], w=["den"])
            tt(P, "dve", ftmp[:], lim, lim, ALU.mult, r=["lam"], w=["ftmp"])
            tt(P, "dve", den[:], den[:], ftmp[:], ALU.add, r=["den", "ftmp"], w=["den"])
            P.dve(lambda e: e.reciprocal(den[:], den[:]), r=["den"], w=["den"])
            ts(P, "dve", am1[:], a_re, -1.0, None, ALU.add, r=["Tre"], w=["am1"])
            tt(P, "dve", fre[:], am1[:], lre, ALU.mult, r=["am1", "lam"], w=["fre"])
            tt(P, "dve", ftmp[:], a_im, lim, ALU.mult, r=["Tim", "lam"], w=["ftmp"])
            tt(P, "dve", fre[:], fre[:], ftmp[:], ALU.add, r=["fre", "ftmp"], w=["fre"])
            tt(P, "dve", fre[:], fre[:], den[:], ALU.mult, r=["fre", "den"], w=["fre"])
            tt(P, "dve", fim[:], a_im, lre, ALU.mult, r=["Tim", "lam"], w=["fim"])
            tt(P, "dve", ftmp[:], am1[:], lim, ALU.mult, r=["am1", "lam"], w=["ftmp"])
            tt(P, "dve", fim[:], fim[:], ftmp[:], ALU.subtract, r=["fim", "ftmp"], w=["fim"])
            tt(P, "dve", fim[:], fim[:], den[:], ALU.mult, r=["fim", "den"], w=["fim"])
            b_re = bt[:, 0].rearrange("p d g h -> p (d g) h"); b_im = bt[:, 1].rearrange("p d g h -> p (d g) h")
            c_re = ctl[:, 0].rearrange("p d g h -> p (d g) h"); c_im = ctl[:, 1].rearrange("p d g h -> p (d g) h")
            fre_b = bc(fre[:].unsqueeze(2), [64, 16, 16]); fim_b = bc(fim[:].unsqueeze(2), [64, 16, 16])
            tt(P, "dve", Bre[:], b_re, fre_b, ALU.mult, r=["bt", "fre"], w=["Bre"])
            tt(P, "dve", btmp[:], b_im, fim_b, ALU.mult, r=["bt", "fim"], w=["btmp"])
            tt(P, "dve", Bre[:], Bre[:], btmp[:], ALU.subtract, r=["Bre", "btmp"], w=["Bre"])
            tt(P, "dve", Bim[:], b_im, fre_b, ALU.mult, r=["bt", "fre"], w=["Bim"])
            tt(P, "dve", btmp[:], b_re, fim_b, ALU.mult, r=["bt", "fim"], w=["btmp"])
            tt(P, "dve", Bim[:], Bim[:], btmp[:], ALU.add, r=["Bim", "btmp"], w=["Bim"])
            a16r = Tre[:, :, 2, 15].rearrange("p (d g) -> p d g", d=2); a16i = Tim[:, :, 2, 15].rearrange("p (d g) -> p d g", d=2)
            for r_ in range(2):
                cp(P, "dve", Ar2[:, :, r_, :], a16r, r=["Tre"], w=["Ar2"])
            ts(P, "dve", AiS[:, :, 0, :], a16i, -1.0, None, ALU.mult, r=["Tim"], w=["AiS"])
            cp(P, "dve", AiS[:, :, 1, :], a16i, r=["Tim"], w=["AiS"])
            P.emit(); esB.close()
            esC = contextlib.ExitStack(); sbC = _sb(nc, esC)
            Z = sbC("Z", [64, 2 * 273 * 16]); Zbf = sbC("Zbf", [64, 2 * 273 * 16], BF16)
            Tsb = sbC("Tsb", [128, 16, 2, 256], BF16); Msb = sbC("Msb", [64, 16, 2, 256], BF16)
            WT = sbC("WT", [128, 16, 2, 2, 64], BF16); Ysb = sbC("Ysb", [128, 2, 272], BF16)
            g1 = [sbC("g1_%d" % i, [64, 16, 16]) for i in range(4)]
            Xre = sbC("Xre", [64, 256]); Xim = sbC("Xim", [64, 256]); Yre = sbC("Yre", [64, 256]); YimN = sbC("YimN", [64, 256])
            Wre = sbC("Wre", [64, 256]); Wim = sbC("Wim", [64, 256])
            t1 = sbC("t1", [64, 2, 16]); t2 = sbC("t2", [64, 2, 16])
            zb = Z[:]
            pst = zb.ap[0][0]

            def zap(a, b_, sub=None, zb=zb, pst=pst):
                off = zb.offset + a * 16
                dstride = (273 + b_ - a) * 16
                if sub is None:
                    return bass.AP(zb.tensor, off, [[pst, 64], [dstride, 2], [1, 16]])
                return bass.AP(zb.tensor, off + 8 * sub, [[pst, 64], [dstride, 2], [1, 8]])

            Z5 = Z[:].rearrange("p (d q r g) -> p d q r g", d=2, q=273, r=2)
            Zb5 = Zbf[:].rearrange("p (d q r g) -> p d q r g", d=2, q=273, r=2)
            P.pool(lambda e: e.memset(Z[:], 0.0), w=["Z"])

            def cprod(eng, ore, oim, are_, aim_, bre_, bim_, negim, r, w):
                tt(P, eng, g1[0][:], are_, bre_, ALU.mult, r=r, w=["g1_0"])
                tt(P, eng, g1[1][:], aim_, bim_, ALU.mult, r=r, w=["g1_1"])
                tt(P, eng, ore, g1[0][:], g1[1][:], ALU.subtract, r=["g1_0", "g1_1"], w=w)
                tt(P, eng, g1[2][:], are_, bim_, ALU.mult, r=r, w=["g1_2"])
                tt(P, eng, g1[3][:], aim_, bre_, ALU.mult, r=r, w=["g1_3"])
                if negim:
                    stt(P, eng, oim, g1[2][:], -1.0, g1[3][:], ALU.mult, ALU.subtract, r=["g1_2", "g1_3"], w=w)
                else:
                    tt(P, eng, oim, g1[2][:], g1[3][:], ALU.add, r=["g1_2", "g1_3"], w=w)

            v3 = lambda t_: t_[:].rearrange("p (a b) -> p a b", b=16)
            for g in range(8):
                for di in range(2):
                    q = di * 8 + g
                    tbx, tby, tbm, tbw = (1, 0, 2, 3) if di == 0 else (0, 1, 4, 0)
                    rd = ["Bre", "Bim", "Tre", "Tim", "ctl"]
                    Bre_b = bc(Bre[:, q, :].unsqueeze(1), [64, 16, 16]); Bim_b = bc(Bim[:, q, :].unsqueeze(1), [64, 16, 16])
                    Cre_b = bc(c_re[:, q, :].unsqueeze(1), [64, 16, 16]); Cim_b = bc(c_im[:, q, :].unsqueeze(1), [64, 16, 16])
                    pw = lambda T_, tb_: bc(T_[:, q, tb_, :].unsqueeze(2), [64, 16, 16])
                    cprod("dve", v3(Xre), v3(Xim), Bre_b, Bim_b, pw(Tre, tbx), pw(Tim, tbx), False, rd, ["Xre"])
                    cprod("dve", v3(Yre), v3(YimN), Cre_b, Cim_b, pw(Tre, tby), pw(Tim, tby), True, rd, ["Yre"])
                    for sh in range(2):
                        pp = ps[2 + sh]; kp = "ps%d" % (2 + sh)
                        mm(P, pp[:, 0:256], Xre[:, sh * 128:(sh + 1) * 128], Yre[:], start=True, stop=False, r=["Xre", "Yre"], w=[kp])
                        mm(P, pp[:, 0:256], Xim[:, sh * 128:(sh + 1) * 128], YimN[:], start=False, stop=True, r=["Xre", "Yre"], w=[kp])
                        tt(P, "dve", Tsb[:, q, sh, :], pp[:, 0:256], mT[:, di, sh, :], ALU.mult, r=[kp, "mT"], w=["Tsb"])
                    cprod("dve", v3(Yre), v3(YimN), Cre_b, Cim_b, pw(Tre, tbm), pw(Tim, tbm), True, rd, ["Yre"])
                    cp(P, "act", Msb[:, q, 0, :], Yre[:], r=["Yre"], w=["Msb"])
                    cp(P, "act", Msb[:, q, 1, :], YimN[:], r=["Yre"], w=["Msb"])
                    cprod("dve", v3(Wre), v3(Wim), Bre_b, Bim_b, pw(Tre, tbw), pw(Tim, tbw), False, rd, ["Wre"])
                    for ri, Wt in enumerate((Wre, Wim)):
                        for sh in range(2):
                            tr(P, ps[4][:, (ri * 2 + sh) * 64:(ri * 2 + sh + 1) * 64], Wt[:, sh * 128:(sh + 1) * 128], k.identf[0:64, 0:64],
                               r=["Wre", "identf"], w=["ps4"])
                    cp(P, "act", WT[:, q].rearrange("p r s c -> p (r s c)"), ps[4][:, 0:256], r=["ps4"], w=["WT"])
                    for ri in range(2):
                        pp = ps[5 + ri]; kp = "ps%d" % (5 + ri)
                        for sh in range(2):
                            mm(P, pp[0:64, 0:272], WT[:, q, ri, sh, :], Ug[:, g, sh, :], start=(sh == 0), stop=(sh == 1), r=["WT", "Ug"], w=[kp])
                        if di == 0:
                            cp(P, "act", Z5[:, 0, 17:273, ri, g], pp[0:64, 0:256], r=[kp], w=["Z"])
                            cp(P, "act", Z5[:, 0, 1:17, ri, g], pp[0:64, 256:272], r=[kp], w=["Z"])
                        else:
                            cp(P, "act", Z5[:, 1, 0:272, ri, g], pp[0:64, 0:272], r=[kp], w=["Z"])
            Ar2v = Ar2[:].rearrange("p d r g -> p d (r g)")
            for kk in range(1, 272):
                prev = zap(kk, 272 - kk); cur = zap(1 + kk, 271 - kk)
                tt(P, "dve", t1[:], prev, Ar2v, ALU.mult, r=["Z", "Ar2"], w=["t1"])
                tt(P, "dve", t2[:, :, 0:8], zap(kk, 272 - kk, 1), AiS[:, :, 0, :], ALU.mult, r=["Z", "AiS"], w=["t2"])
                tt(P, "dve", t2[:, :, 8:16], zap(kk, 272 - kk, 0), AiS[:, :, 1, :], ALU.mult, r=["Z", "AiS"], w=["t2"])
                tt(P, "dve", cur, cur, t1[:], ALU.add, r=["Z", "t1"], w=["Z"])
                tt(P, "dve", cur, cur, t2[:], ALU.add, r=["Z", "t2"], w=["Z"])
            cp(P, "act", Zbf[:], Z[:], r=["Z"], w=["Zbf"])
            for g in range(8):
                for th in range(2):
                    pp = ps[th]; kp = "ps%d" % th
                    first = True
                    for di in range(2):
                        q = di * 8 + g
                        for sh in range(2):
                            if (di == 0 and sh <= th) or (di == 1 and sh >= th):
                                mm(P, pp[:, 0:272], Tsb[:, q, sh, th * 128:(th + 1) * 128], Ug[:, g, sh, :], start=first, stop=False,
                                   r=["Tsb", "Ug"], w=[kp])
                                first = False
                    for di in range(2):
                        q = di * 8 + g
                        for ri in range(2):
                            lhs = Msb[:, q, ri, th * 128:(th + 1) * 128]
                            last = (di == 1 and ri == 1)
                            if di == 0:
                                mm(P, pp[:, 0:256], lhs, Zb5[:, 0, 16:272, ri, g], start=False, stop=last, r=["Msb", "Zbf"], w=[kp])
                                mm(P, pp[:, 256:272], lhs, Zb5[:, 0, 0:16, ri, g], start=False, stop=last, r=["Msb", "Zbf"], w=[kp])
                            else:
                                mm(P, pp[:, 0:256], lhs, Zb5[:, 1, 1:257, ri, g], start=False, stop=last, r=["Msb", "Zbf"], w=[kp])
                                mm(P, pp[:, 256:272], lhs, Zb5[:, 1, 257:273, ri, g], start=False, stop=last, r=["Msb", "Zbf"], w=[kp])
                    cp(P, "act", Ysb[:, th, :], pp[:, 0:272], r=[kp], w=["Ysb"])
                for ct, (tb, M, col0) in enumerate(CT):
                    for th in range(2):
                        tr(P, psb[0:M, th * 128:(th + 1) * 128], Ysb[:, th, col0:col0 + M], k.identb[:], r=["Ysb", "identb"], w=["psb"])
                    cp(P, "dve", Yp[0:M, ct, :, g * 16:(g + 1) * 16], psb[0:M, 0:256].rearrange("p (t h) -> p t h", h=16), r=["psb"], w=["Yp"])
            P.emit(); esC.close()
            esD = contextlib.ExitStack(); sbD = _sb(nc, esD)
            s32 = sbD("s32", [128, 16, 128]); gx = sbD("gx", [128, 16, 128]); gq = sbD("gq", [128, 16, 128])
            abf = sbD("abf", [128, 16, 128], BF16); aTt = sbD("aTt", [128, 2048], BF16)
            for ct, (tb, M, col0) in enumerate(CT):
                dsl = bc(dbc[0:M, fc * 128:(fc + 1) * 128].unsqueeze(1), [M, 16, 128])
                tt(P, "pool", s32[0:M], Uf[0:M, ct], dsl, ALU.mult, r=["Uf", "dbc"], w=["s32"])
                tt(P, "dve", s32[0:M], s32[0:M], Yp[0:M, ct], ALU.add, r=["s32", "Yp"], w=["s32"])
                if k.debug and "dbg_s" in k.d:
                    P.dma(k.d["dbg_s"][tb:tb + M * 16, fc * 128:(fc + 1) * 128].rearrange("(c s) n -> c s n", s=16), s32[0:M], r=["s32"])
                tt(P, "pool", gx[0:M], s32[0:M], s32[0:M], ALU.mult, r=["s32"], w=["gx"])
                ts(P, "dve", gx[0:M], gx[0:M], 0.044715, 1.0, ALU.mult, ALU.add, r=["gx"], w=["gx"])
                tt(P, "pool", gq[0:M], gx[0:M], s32[0:M], ALU.mult, r=["gx", "s32"], w=["gq"])
                actf(P, gq[0:M], gq[0:M], AF.Sigmoid, r=["gq"], w=["gq"], scale=1.5957691216057308)
                tt(P, "dve", abf[0:M], s32[0:M], gq[0:M], ALU.mult, r=["s32", "gq"], w=["abf"])
                aT3 = aTt[:, 0:M * 16].rearrange("p (c t) -> p t c", t=16)
                for t0 in range(0, 16, 8):
                    for t_ in range(t0, t0 + 8):
                        tr(P, psb[:, (t_ - t0) * 128:(t_ - t0) * 128 + M], abf[0:M, t_, :], k.identb[0:M, 0:M], r=["abf", "identb"], w=["psb"])
                    cp(P, "act", aT3[:, t0:t0 + 8, :], psb[:].rearrange("p (t c) -> p t c", c=128)[:, :, 0:M], r=["psb"], w=["aTt"])
                P.dma(k.d["s5oT"][:, fc, tb:tb + M * 16], aTt[:, 0:M * 16], r=["aTt"], w=["s5oT_a"])
            P.emit(); esD.close()
    with contextlib.ExitStack() as es:
        sb = _sb(nc, es)
        gw = sb("gw", [128, 4, 512], BF16); gb = sb("gb", [128, 4])
        P.dma(gw[:], k.d["glu_w"].rearrange("(c p) n -> p c n", p=128), w=["gw"], q="pool")
        P.dma(gb[:], k.d["glu_b"], w=["gb"])
        aT = [sb("aT%d" % i, [128, 4, 512], BF16) for i in range(2)]
        sg_ = [sb("sgz%d" % i, [128, 4, 512]) for i in range(2)]
        oT = [sb("oT%d" % i, [128, 4, 512], BF16) for i in range(2)]
        for bi, t0 in enumerate(range(0, T, 512)):
            n = min(512, T - t0)
            b = bi % 2
            a_, ka = aT[b], "aT%d" % b
            P.dma(a_[:, :, 0:n], k.d["s5oT"][:, :, t0:t0 + n], w=[ka])
            for oc in range(4):
                pp = ps[oc]; kp = "ps%d" % oc
                for kc in range(4):
                    mm(P, pp[:, 0:n], gw[:, kc, oc * 128:(oc + 1) * 128], a_[:, kc, 0:n], start=(kc == 0), stop=(kc == 3), r=["gw", ka], w=[kp])
                actf(P, sg_[b][:, oc, 0:n], pp[:, 0:n], AF.Sigmoid, r=[kp], w=["sgz%d" % b], sr=["gb"], bias=gb[:, oc:oc + 1])
            tt(P, "dve", oT[b][:, :, 0:n], a_[:, :, 0:n], sg_[b][:, :, 0:n], ALU.mult, r=[ka, "sgz%d" % b], w=["oT%d" % b])
            P.dma(k.d["s5oT"][:, :, t0:t0 + n], oT[b][:, :, 0:n], r=["oT%d" % b], w=["s5oT_%d" % bi])
        P.emit()
```
